# Optimizing a Trainium2 kernel written in Bass

```python
import jax, jax.numpy as jnp
from jax import lax
import numpy as np

D_MODEL = 1024
BATCH = 4
SEQ = 8192
DEPTH = 1

D_MIX = D_MODEL
HEAD_DIM = 64
ATTN_WIDTH = D_MIX // 2
N_Q_HEADS = ATTN_WIDTH // HEAD_DIM
N_KV_HEADS = 2
Q_PER_KV = N_Q_HEADS // N_KV_HEADS
KV_WIDTH = N_KV_HEADS * HEAD_DIM
WINDOW = 128
ATTN_BLOCK = 128
GMLP_WIDTH = D_MIX - ATTN_WIDTH
N_GMLP_HEADS = GMLP_WIDTH // HEAD_DIM
GMLP_CHUNK = 128
IN_PROJ = ATTN_WIDTH + 2 * KV_WIDTH + 2 * GMLP_WIDTH
N_EXPERT_GROUPS = 4
EXPERTS_PER_GROUP = 8
N_EXPERTS = N_EXPERT_GROUPS * EXPERTS_PER_GROUP
TOP_K_IN_GROUP = 2
D_EXPERT = 256
RMS_EPS = 1e-6
LN_EPS = 1e-5
NEG_INF = -1e30

kernel_name = "hymba_swa_sink_gmlp_hiermoe_block"


def rmsnorm(x, g):
    xf = x.astype(jnp.float32)
    y = xf * lax.rsqrt(jnp.mean(xf * xf, axis=-1, keepdims=True) + RMS_EPS)
    return (y * g.astype(jnp.float32)).astype(x.dtype)


def layernorm(x, g, b):
    xf = x.astype(jnp.float32)
    mu = jnp.mean(xf, axis=-1, keepdims=True)
    var = jnp.mean(jnp.square(xf - mu), axis=-1, keepdims=True)
    y = (xf - mu) * lax.rsqrt(var + LN_EPS)
    return (y * g.astype(jnp.float32) + b.astype(jnp.float32)).astype(x.dtype)


def sliding_window_attention(q, k, v, sinks):
    B, S = q.shape[0], q.shape[1]
    nb = S // ATTN_BLOCK
    qb = q.reshape(B, nb, ATTN_BLOCK, N_KV_HEADS, Q_PER_KV, HEAD_DIM)
    kb = k.reshape(B, nb, ATTN_BLOCK, N_KV_HEADS, HEAD_DIM)
    vb = v.reshape(B, nb, ATTN_BLOCK, N_KV_HEADS, HEAD_DIM)
    pad = ((0, 0), (1, 0), (0, 0), (0, 0), (0, 0))
    k_band = jnp.concatenate([jnp.pad(kb[:, :-1], pad), kb], axis=2)
    v_band = jnp.concatenate([jnp.pad(vb[:, :-1], pad), vb], axis=2)
    scale = HEAD_DIM ** -0.5
    scores = jnp.einsum('bnqkgd,bnskd->bnkgqs', qb, k_band).astype(jnp.float32) * scale
    q_loc = jnp.arange(ATTN_BLOCK)[:, None]
    k_off = jnp.arange(2 * ATTN_BLOCK)[None, :] - ATTN_BLOCK
    diff = q_loc - k_off
    in_window = (diff >= 0) & (diff < WINDOW)
    k_abs = jnp.arange(nb)[:, None, None] * ATTN_BLOCK + k_off[None]
    mask = in_window[None] & (k_abs >= 0)
    scores = jnp.where(mask[None, :, None, None], scores, NEG_INF)
    sink = sinks.astype(jnp.float32).reshape(N_KV_HEADS, Q_PER_KV)[None, None, :, :, None, None]
    m = jnp.maximum(jnp.max(scores, axis=-1, keepdims=True), sink)
    p = jnp.exp(scores - m)
    denom = jnp.sum(p, axis=-1, keepdims=True) + jnp.exp(sink - m)
    probs = (p / denom).astype(v.dtype)
    out = jnp.einsum('bnkgqs,bnskd->bnqkgd', probs, v_band)
    return out.reshape(B, S, ATTN_WIDTH)


def chunked_spatial_gating(u, v, w_s, b_s, ln_g, ln_b):
    B, S = u.shape[0], u.shape[1]
    nc = S // GMLP_CHUNK
    u = jax.nn.gelu(u, approximate=False)
    v = layernorm(jax.nn.gelu(v, approximate=False), ln_g, ln_b)
    vc = v.reshape(B, nc, GMLP_CHUNK, N_GMLP_HEADS, HEAD_DIM)
    causal = jnp.tril(jnp.ones((GMLP_CHUNK, GMLP_CHUNK), dtype=bool))
    w = jnp.where(causal[None], w_s, 0).astype(v.dtype)
    s = jnp.einsum('htc,bnchd->bnthd', w, vc) + b_s.T.astype(v.dtype)[None, None, :, :, None]
    return u * s.reshape(B, S, GMLP_WIDTH)


def hierarchical_moe(x, w_gr, b_gr, w_er, b_er, w_gate, w_up, w_down):
    B, S, D = x.shape
    t = x.reshape(B * S, D)
    g_logits = (t @ w_gr).astype(jnp.float32) + b_gr.astype(jnp.float32)
    g_prob = jax.nn.softmax(g_logits, axis=-1)
    g_idx = jnp.argmax(g_logits, axis=-1)
    g_w = jnp.take_along_axis(g_prob, g_idx[:, None], axis=-1)
    e_all = jnp.einsum('td,gde->tge', t, w_er).astype(jnp.float32) + b_er.astype(jnp.float32)
    e_logits = jnp.take_along_axis(e_all, g_idx[:, None, None], axis=1)[:, 0]
    top_v, top_i = lax.top_k(e_logits, TOP_K_IN_GROUP)
    e_w = jax.nn.softmax(top_v, axis=-1)
    within = jnp.sum(jax.nn.one_hot(top_i, EXPERTS_PER_GROUP, dtype=jnp.float32) * e_w[..., None], axis=1)
    gates = jax.nn.one_hot(g_idx, N_EXPERT_GROUPS, dtype=jnp.float32)[:, :, None] * within[:, None, :] * g_w[:, :, None]
    gates = gates.reshape(B * S, N_EXPERTS).astype(t.dtype)
    out = jnp.zeros_like(t)
    for e in range(N_EXPERTS):
        hid = jax.nn.silu(t @ w_gate[e]) * (t @ w_up[e])
        out = out + gates[:, e:e + 1] * (hid @ w_down[e])
    return out.reshape(B, S, D)


def setup_inputs(seed: int = 0) -> dict:
    key = jax.random.key(seed)
    ks = jax.random.split(key, 20)
    f32 = jnp.float32
    nrm = lambda k, shape, s: (jax.random.normal(k, shape, f32) * s)
    L = DEPTH
    return {
        "x": nrm(ks[0], (BATCH, SEQ, D_MODEL), 1.0),
        "mix_norm_g": 1.0 + nrm(ks[1], (L, D_MODEL), 0.01),
        "w_in": nrm(ks[2], (L, D_MODEL, IN_PROJ), D_MODEL ** -0.5),
        "attn_sinks": nrm(ks[3], (L, N_Q_HEADS), 1.0),
        "w_spatial": nrm(ks[4], (L, N_GMLP_HEADS, GMLP_CHUNK, GMLP_CHUNK), GMLP_CHUNK ** -0.5),
        "b_spatial": 1.0 + nrm(ks[5], (L, N_GMLP_HEADS, GMLP_CHUNK), 0.1),
        "gmlp_ln_g": 1.0 + nrm(ks[6], (L, GMLP_WIDTH), 0.01),
        "gmlp_ln_b": nrm(ks[7], (L, GMLP_WIDTH), 0.01),
        "attn_out_g": 1.0 + nrm(ks[8], (L, ATTN_WIDTH), 0.01),
        "gmlp_out_g": 1.0 + nrm(ks[9], (L, GMLP_WIDTH), 0.01),
        "w_out": nrm(ks[10], (L, D_MIX, D_MODEL), D_MIX ** -0.5),
        "ffn_norm_g": 1.0 + nrm(ks[11], (L, D_MODEL), 0.01),
        "w_group_router": nrm(ks[12], (L, D_MODEL, N_EXPERT_GROUPS), D_MODEL ** -0.5),
        "b_group_router": nrm(ks[13], (L, N_EXPERT_GROUPS), 0.01),
        "w_expert_router": nrm(ks[14], (L, N_EXPERT_GROUPS, D_MODEL, EXPERTS_PER_GROUP), D_MODEL ** -0.5),
        "b_expert_router": nrm(ks[15], (L, N_EXPERT_GROUPS, EXPERTS_PER_GROUP), 0.01),
        "w_gate": nrm(ks[16], (L, N_EXPERTS, D_MODEL, D_EXPERT), D_MODEL ** -0.5),
        "w_up": nrm(ks[17], (L, N_EXPERTS, D_MODEL, D_EXPERT), D_MODEL ** -0.5),
        "w_down": nrm(ks[18], (L, N_EXPERTS, D_EXPERT, D_MODEL), D_EXPERT ** -0.5),
        "final_norm_g": 1.0 + nrm(ks[19], (D_MODEL,), 0.01),
    }


def reference(x, mix_norm_g, w_in, attn_sinks, w_spatial, b_spatial, gmlp_ln_g, gmlp_ln_b,
              attn_out_g, gmlp_out_g, w_out, ffn_norm_g, w_group_router, b_group_router,
              w_expert_router, b_expert_router, w_gate, w_up, w_down, final_norm_g):
    B, S = x.shape[0], x.shape[1]
    splits = [ATTN_WIDTH, ATTN_WIDTH + KV_WIDTH, ATTN_WIDTH + 2 * KV_WIDTH,
              ATTN_WIDTH + 2 * KV_WIDTH + GMLP_WIDTH]
    h = x
    for l in range(DEPTH):
        hn = rmsnorm(h, mix_norm_g[l])
        proj = hn @ w_in[l]
        q, k, v, gu, gv = jnp.split(proj, splits, axis=-1)
        q = q.reshape(B, S, N_Q_HEADS, HEAD_DIM)
        k = k.reshape(B, S, N_KV_HEADS, HEAD_DIM)
        v = v.reshape(B, S, N_KV_HEADS, HEAD_DIM)
        a = sliding_window_attention(q, k, v, attn_sinks[l])
        g = chunked_spatial_gating(gu, gv, w_spatial[l], b_spatial[l], gmlp_ln_g[l], gmlp_ln_b[l])
        mixed = jnp.concatenate([rmsnorm(a, attn_out_g[l]), rmsnorm(g, gmlp_out_g[l])], axis=-1)
        h = h + mixed @ w_out[l]
        hn = rmsnorm(h, ffn_norm_g[l])
        h = h + hierarchical_moe(hn, w_group_router[l], b_group_router[l], w_expert_router[l],
                                 b_expert_router[l], w_gate[l], w_up[l], w_down[l])
    return rmsnorm(h, final_norm_g)
```

```python
import contextlib
import numpy as np
import concourse.bass as bass
import concourse.mybir as mybir
from concourse.bass_utils import run_bass_kernel_spmd

F32 = mybir.dt.float32
BF16 = mybir.dt.bfloat16
I32 = mybir.dt.int32
AF = mybir.ActivationFunctionType
ALU = mybir.AluOpType
AX = mybir.AxisListType

NCORES = 8
D = 1024
TOK = 4096
NBLK = TOK // 128
NE = 32
DE = 256
INP = 1792
RS_EPS = 1e-6
LN_EPS = 1e-5
MAGIC = 1597463007


PSUM_KEYS = frozenset(["pA", "pB", "pC", "pD", "pE", "pF", "pG", "pH"])


class Op:
    __slots__ = ("eng", "fn", "deps", "dma", "seq", "sig", "sem", "count", "region")

    def __init__(self, eng, fn, dma):
        self.eng = eng
        self.fn = fn
        self.dma = dma
        self.deps = []
        self.seq = 0
        self.sig = False
        self.sem = None
        self.count = 0


class Region:
    def __init__(self, thresh, parent, regs=None, invert=False):
        self.thresh = thresh
        self.regs = regs
        self.invert = invert
        self.items = []
        self.parent = parent
        self.end = {}


class Sched:
    def __init__(self, nc, es):
        self.nc = nc
        self.es = es
        self.engs = {"sp": nc.sync, "act": nc.scalar, "dve": nc.vector, "pe": nc.tensor, "pool": nc.gpsimd}
        self.esem = {e: es.enter_context(nc.semaphore("sem_" + e)) for e in ("act", "dve", "pe", "pool")}
        self.ops = []
        self.root = Region(None, None)
        self.cur = self.root
        self.regs = None
        self.suffix = ""
        self.lw = {}
        self.rd = {}
        self.dsem = {}

    def add(self, eng, fn, r=(), w=(), dma=None):
        if self.suffix:
            r = [k if self.is_global(k) else k + self.suffix for k in r]
            w = [k if self.is_global(k) else k + self.suffix for k in w]
        op = Op(eng, fn, dma)
        pr = [k for k in r if k in PSUM_KEYS]
        if pr:
            r = [k for k in r if k not in PSUM_KEYS]
            w = list(w) + pr
        deps = []
        for k in r:
            x = self.lw.get(k)
            if x is not None:
                deps.append(x)
        for k in w:
            x = self.lw.get(k)
            if x is not None:
                deps.append(x)
            deps.extend(self.rd.get(k, ()))
        op.deps = deps
        for k in r:
            self.rd.setdefault(k, []).append(op)
        for k in w:
            self.lw[k] = op
            self.rd[k] = []
        op.region = self.cur
        self.ops.append(op)
        self.cur.items.append(op)
        return op

    GLOBAL_KEYS = frozenset(["win", "wout", "identb", "identf", "g1b", "gmb0", "gmb1", "g2b", "gfb", "lngb", "lnbb", "bsb",
                             "esink", "wsT", "msk", "wr0", "wr1", "wr2", "wr3", "wr4", "rb0", "rb1", "trilf"])
    GLOBAL_PREFIXES = ("kT", "vaug", "xt", "h1t", "lg_", "h1d_", "hn2d_")

    def is_global(self, k):
        return k in PSUM_KEYS or k in self.GLOBAL_KEYS or k.startswith(self.GLOBAL_PREFIXES)

    def begin_if(self, thresh, regs=None, invert=False):
        r = Region(thresh, self.cur, regs if regs is not None else self.regs, invert)
        self.cur.items.append(r)
        self.cur = r

    def end_if(self):
        self.cur = self.cur.parent

    def emit(self):
        nc = self.nc
        import os
        mx = int(os.environ.get("K_MAXOPS", "0"))
        if mx:
            self.ops = self.ops[:mx]
        if os.environ.get("K_LISTOPS"):
            for i, op in enumerate(self.ops):
                print(i, op.eng, op.dma, getattr(op, "tag", ""))
        for op in self.ops:
            for d in op.deps:
                if d.dma is None:
                    d.sig = True
        cnt = {e: 0 for e in self.esem}
        dcnt = {}
        for op in self.ops:
            if op.dma is not None:
                if op.dma not in self.dsem:
                    self.dsem[op.dma] = self.es.enter_context(nc.semaphore("dsem_" + op.dma))
                    dcnt[op.dma] = 0
                dcnt[op.dma] += 16
                op.sem = self.dsem[op.dma]
                op.count = dcnt[op.dma]
            elif op.sig:
                cnt[op.eng] += 1
                op.seq = cnt[op.eng]
        def chain(reg):
            out = []
            while reg is not None and reg.parent is not None:
                out.append(reg)
                reg = reg.parent
            return out

        for op in self.ops:
            if op.dma is not None:
                key, val = ("d", op.dma), op.count
            elif op.sig:
                key, val = ("e", op.eng), op.seq
            else:
                continue
            for rg in chain(op.region):
                if rg.end.get(key, 0) < val:
                    rg.end[key] = val

        def dep_wait(d, waiter_region):
            if d.dma is not None:
                key, val, sem = ("d", d.dma), d.count, d.sem
            else:
                key, val, sem = ("e", d.eng), d.seq, self.esem[d.eng]
            mine = set(id(r) for r in chain(waiter_region))
            for rg in reversed(chain(d.region)):
                if id(rg) not in mine:
                    val = rg.end[key]
                    break
            return key, val, sem

        waited = {e: {} for e in self.engs}
        first_cnt = {}

        def emit_op(op):
            E = self.engs[op.eng]
            need = {}
            for d in op.deps:
                if d.dma is None and d.eng == "pe" and op.eng == "pe" and op.dma is None:
                    continue
                key, val, sem = dep_wait(d, op.region)
                if key not in need or need[key][1] < val:
                    need[key] = (sem, val)
            for key, (sem, val) in need.items():
                if waited[op.eng].get(key, 0) >= val:
                    continue
                waited[op.eng][key] = val
                E.wait_ge(sem, val)
            ins = op.fn()
            if op.dma is not None:
                ins.then_inc(op.sem, 16)
            elif op.sig:
                ins.then_inc(self.esem[op.eng], 1)

        def flat(items):
            for it in items:
                if isinstance(it, Region):
                    yield from flat(it.items)
                else:
                    yield it

        def compensate(region):
            esig = {}
            dsig = {}
            inside = set(id(o) for o in flat(region.items))
            needs = {}
            for op in flat(region.items):
                for d in op.deps:
                    if id(d) in inside:
                        continue
                    key, val, sem = dep_wait(d, region.parent)
                    nd = needs.setdefault(op.eng, {})
                    if key not in nd or nd[key][1] < val:
                        nd[key] = (sem, val)
            for eng, nd in needs.items():
                for key, (sem, val) in nd.items():
                    if waited[eng].get(key, 0) >= val:
                        continue
                    self.engs[eng].wait_ge(sem, val)
            for op in flat(region.items):
                if op.dma is not None:
                    d = dsig.setdefault(op.dma, [op.eng, op.count - 16, 0, op.sem])
                    d[2] += 16
                elif op.sig:
                    e = esig.setdefault(op.eng, [op.seq - 1, 0])
                    e[1] += 1
            for eng, (before, n) in esig.items():
                E = self.engs[eng]
                if before > 0:
                    E.wait_ge(self.esem[eng], before)
                while n > 0:
                    c = min(n, 112)
                    n -= c
                    E.sem_inc(self.esem[eng], c)
            for k, (eng, before, n, sem) in dsig.items():
                E = self.engs[eng]
                if before > 0:
                    E.wait_ge(sem, before)
                if eng == "pool":
                    while n > 0:
                        c = min(n, 240)
                        n -= c
                        i = self.ndummy
                        self.ndummy += 1
                        E.dma_start(out=self.dummy_d[0:1, i:i + 1], in_=self.dummy[0:1, 0:1]).then_inc(sem, c)
                else:
                    while n > 0:
                        c = min(n, 112)
                        n -= c
                        E.sem_inc(sem, c)

        def emit_items(items):
            for it in items:
                if isinstance(it, Region):
                    snap = {e: dict(v) for e, v in waited.items()}
                    if it.invert:
                        with nc.If_lt(it.regs, it.thresh + 1):
                            emit_items(it.items)
                        with nc.Else():
                            compensate(it)
                    else:
                        with nc.If_lt(it.regs, it.thresh + 1):
                            compensate(it)
                        with nc.Else():
                            emit_items(it.items)
                    for e in waited:
                        waited[e] = snap[e]
                else:
                    emit_op(it)

        emit_items(self.root.items)
        sp = nc.sync
        for k, sem in self.dsem.items():
            sp.wait_ge(sem, dcnt[k])
        return cnt

    def tt(self, eng, out, in0, in1, op, r, w):
        E = self.engs[eng]
        return self.add(eng, lambda: E.tensor_tensor(out=out, in0=in0, in1=in1, op=op), r, w)

    def ts(self, eng, out, in0, s1, s2, op0, op1, r, w):
        E = self.engs[eng]
        if op1 is None:
            return self.add(eng, lambda: E.tensor_scalar(out=out, in0=in0, scalar1=s1, scalar2=None, op0=op0), r, w)
        return self.add(eng, lambda: E.tensor_scalar(out=out, in0=in0, scalar1=s1, scalar2=s2, op0=op0, op1=op1), r, w)

    def stt(self, eng, out, in0, scalar, in1, op0, op1, r, w):
        E = self.engs[eng]
        return self.add(eng, lambda: E.scalar_tensor_tensor(out=out, in0=in0, scalar=scalar, in1=in1, op0=op0, op1=op1), r, w)

    def copy(self, eng, out, in_, r, w):
        E = self.engs[eng]
        if eng == "act":
            return self.add(eng, lambda: E.copy(out=out, in_=in_), r, w)
        return self.add(eng, lambda: E.tensor_copy(out=out, in_=in_), r, w)

    def actv(self, out, in_, func, r, w, scale=1.0, accum=None):
        E = self.nc.scalar
        if accum is None:
            return self.add("act", lambda: E.activation(out=out, in_=in_, func=func, scale=scale), r, w)
        return self.add("act", lambda: E.activation(out=out, in_=in_, func=func, scale=scale, accum_out=accum), r, w)

    def red(self, eng, out, in_, op, r, w):
        E = self.engs[eng]
        return self.add(eng, lambda: E.tensor_reduce(out=out, in_=in_, axis=AX.X, op=op), r, w)

    def mm(self, lst, r, w):
        T = self.nc.tensor

        def f():
            ins = None
            for (out, lhsT, rhs, st, sp) in lst:
                ins = T.matmul(out, lhsT=lhsT, rhs=rhs, start=st, stop=sp)
            return ins
        return self.add("pe", f, r, w)

    def tr(self, lst, r, w):
        T = self.nc.tensor

        def f():
            ins = None
            for (out, in_, ident) in lst:
                ins = T.transpose(out=out, in_=in_, identity=ident)
            return ins
        return self.add("pe", f, r, w)

    def dma(self, q, out, in_, r, w, stream, ncok=False):
        E = self.engs[q]
        if ncok:
            return self.add(q, lambda: E.dma_start(out=out, in_=in_, allow_slow_non_contiguous=True), r, w, dma=stream)
        return self.add(q, lambda: E.dma_start(out=out, in_=in_), r, w, dma=stream)


def build_nc(TOK=TOK, NE_RUN=NE):
    NBLK = TOK // 128
    BPP = NBLK
    CAP = TOK
    JMAX = CAP // 128
    nc = bass.Bass("TRN2", target_bir_lowering=False)

    def din(name, shape, dt=F32):
        return nc.dram_tensor(name, list(shape), dt, kind="ExternalInput").ap()

    x_d = din("x", [TOK, D])
    xh_d = din("xh", [128, D])
    masks_d = din("masks", [128, 4, 512])
    g1_d = din("mix_norm_g", [1, D])
    win_d = din("w_in", [D, INP])
    sinks_d = din("attn_sinks", [1, 8])
    wsp_d = din("w_spatial", [8, 128, 128])
    bsp_d = din("b_spatial", [8, 128])
    lng_d = din("gmlp_ln_g", [1, 512])
    lnb_d = din("gmlp_ln_b", [1, 512])
    ga_d = din("attn_out_g", [1, 512])
    gg_d = din("gmlp_out_g", [1, 512])
    wout_d = din("w_out", [D, D])
    g2_d = din("ffn_norm_g", [1, D])
    wgr_d = din("w_group_router", [D, 4])
    bgr_d = din("b_group_router", [1, 4])
    wer_d = din("w_expert_router", [4, D, 8])
    ber_d = din("b_expert_router", [1, 32])
    wg_d = din("w_gate", [NE, D, DE])
    wu_d = din("w_up", [NE, D, DE])
    wd_d = din("w_down", [NE, DE, D])
    gf_d = din("final_norm_g", [1, D])
    tokid_d = din("tokid", [128, NBLK], I32)
    ecap_d = din("ecap", [1, 32])
    dst_d = din("dst", [128, NBLK, 2], I32)
    out_d = nc.dram_tensor("out", [TOK, D], F32, kind="ExternalOutput").ap()
    h1_d = nc.dram_tensor("h1_scr", [TOK, D], F32, kind="Internal").ap()
    hn2_d = nc.dram_tensor("hn2_scr", [TOK, D], BF16, kind="Internal").ap()
    info_d = nc.dram_tensor("info_scr", [NE * CAP, 4], I32, kind="Internal").ap()
    wgb_d = nc.dram_tensor("wg_bf", [NE, D, DE], BF16, kind="Internal").ap()
    wub_d = nc.dram_tensor("wu_bf", [NE, D, DE], BF16, kind="Internal").ap()
    wdb_d = nc.dram_tensor("wd_bf", [NE, DE, D], BF16, kind="Internal").ap()
    y2_d = nc.dram_tensor("y2_scr", [2 * TOK, D], F32, kind="Internal").ap()

    es = contextlib.ExitStack()
    with es:
        def sb(name, shape, dt):
            return es.enter_context(nc.sbuf_tensor(name, list(shape), dt))

        def psum(name, shape, dt):
            return es.enter_context(nc.psum_tensor(name, list(shape), dt))

        S = Sched(nc, es)
        S.dummy = sb("dmy_sb", [128, 2], F32)
        S.dummy_d = nc.dram_tensor("dmy_scr", [1, 8192], F32, kind="Internal").ap()
        S.ndummy = 0

        win = sb("win", [128, 8, INP], BF16)
        wout = sb("wout", [128, 8, D], BF16)
        wgt = [sb(f"wgt{i}", [128, 8, DE], BF16) for i in range(2)]
        wup = [sb(f"wup{i}", [128, 8, DE], BF16) for i in range(2)]
        wdn = [sb(f"wdn{i}", [128, 2, D], BF16) for i in range(2)]
        g1b = sb("g1b", [128, D], F32)
        gmb = sb("gmb", [128, D], F32)
        g2b = sb("g2b", [128, D], F32)
        gfb = sb("gfb", [128, D], F32)
        lngb = sb("lngb", [128, 512], F32)
        lnbb = sb("lnbb", [128, 512], F32)
        bsb = sb("bsb", [128, 8], F32)
        esink = sb("esink", [128, 8], F32)
        wsT = sb("wsT", [128, 8, 128], BF16)
        msk = sb("msk", [128, 4, 512], BF16)
        trilf = sb("trilf", [128, 128], F32)
        identb = sb("identb", [128, 128], BF16)
        identf = sb("identf", [128, 128], F32)
        wr = sb("wr", [128, 8, 36], F32)
        rbias = sb("rbias", [128, 36], F32)

        h1t = [sb(f"h1t{i}", [128, D], F32) for i in range(2)]
        gates = sb("gates", [128, BPP, 32], F32)
        lg = sb("lg", [128, BPP, 36], F32)
        Mb = sb("Mb", [128, BPP, 32], BF16)
        Macc = sb("Macc", [128, BPP + 1, 32], BF16)
        slf = sb("slf", [128, BPP, 32], F32)
        tokid = sb("tokid_sb", [128, BPP], I32)
        ecap = sb("ecap_sb", [128, 32], F32)
        infoA = sb("infoA", [128, BPP, 4], F32)
        infoB = sb("infoB", [128, BPP, 4], F32)
        dstt = sb("dst_sb", [128, BPP, 2], I32)
        sAi = sb("sAi", [128, BPP], I32)
        sBi = sb("sBi", [128, BPP], I32)
        cnt_i = sb("cnt_i", [128, 32], I32)
        onesb = sb("onesb", [128, 128], BF16)
        utri = sb("utri", [128, 128], BF16)
        si = [sb(f"si{i}", [128, 4], I32) for i in range(8)]
        kT = sb("kT", [128, 3, 128], BF16)
        vaug = sb("vaug", [128, 3, 2, 65], BF16)

        xt = [sb(f"xt{i}", [128, D], F32) for i in range(2)]
        JUNK = [sb(f"junk{i}", [128, D], BF16) for i in range(2)]
        HN = [sb(f"hn{i}", [128, D], BF16) for i in range(2)]
        HNT = [sb(f"hnT{i}", [128, 8, 128], BF16) for i in range(2)]
        QT = [sb(f"qT{i}", [128, 4, 128], BF16) for i in range(2)]
        UT = [sb(f"u_t{i}", [128, 512], F32) for i in range(2)]
        VGT = [sb(f"vg_t{i}", [128, 512], F32) for i in range(2)]
        VNT = [sb(f"vn_t{i}", [128, 512], BF16) for i in range(2)]
        PT = [[[sb(f"p{i}{g}{kb}", [128, 4, 128], BF16) for kb in range(2)] for g in range(2)] for i in range(2)]
        AT = [sb(f"a_t{i}", [128, 8, 64], F32) for i in range(2)]
        GMT = [sb(f"gm_t{i}", [128, 8, 64], F32) for i in range(2)]
        MIXED = [sb(f"mixed{i}", [128, D], BF16) for i in range(2)]
        MIXEDT = [sb(f"mixedT{i}", [128, 8, 128], BF16) for i in range(2)]
        HN2 = [sb(f"hn2_{i}", [128, D], F32) for i in range(2)]
        HN2T32 = [sb(f"hn2T32_{i}", [128, 8, 128], F32) for i in range(2)]
        junk = JUNK[0]
        wsf = HN2[0][:].rearrange("p (h c) -> p h c", h=8)
        wsm = MIXED[0][:].rearrange("p (h c) -> p h c", h=8)
        hid = [sb(f"hid{i}", [128, 2, 128], BF16) for i in range(3)]
        sil = [sb(f"sil{i}", [128, 256], F32) for i in range(3)]
        woutf = wout[:].rearrange("p k n -> p (k n)")
        xg = [woutf[:, i * 1024:(i + 1) * 1024] for i in range(2)]
        xgT = [woutf[:, 2048 + i * 1024:2048 + (i + 1) * 1024].rearrange("p (k t) -> p k t", k=8) for i in range(2)]
        yt = [woutf[:, 4096 + i * 2048:4096 + (i + 1) * 2048].bitcast(F32) for i in range(2)]
        winf = win[:].rearrange("p k n -> p (k n)").bitcast(F32)
        ya = [winf[:, i * 1024:(i + 1) * 1024] for i in range(2)]
        yt = yt + [winf[:, i * 1024:(i + 1) * 1024] for i in range(2)]
        yt = yt + [winf[:, 2048:3072]]
        xg = xg + [winf[:, 3072:3584].bitcast(BF16)]
        xgT = xgT + [winf[:, 3584:4096].bitcast(BF16).rearrange("p (k t) -> p k t", k=8)]
        rt = winf[:, 2048:2048 + BPP * 96].rearrange("p (b c) -> p b c", c=96)
        yc = [winf[:, i * 2048:(i + 1) * 2048].rearrange("p (k d) -> p k d", k=2) for i in range(3)]
        woutf32 = woutf.bitcast(F32)
        hc = [woutf32[:, i * 1024:(i + 1) * 1024] for i in range(3)]
        STT = [sb(f"st{i}", [128, 64], F32) for i in range(2)]
        st = STT[0]

        pA32 = psum("pA", [128, 512], F32)
        pA = pA32[:].bitcast(BF16).rearrange("p (k t) -> p k t", k=8)
        pB = psum("pB", [128, 512], F32)
        pC = psum("pC", [128, 512], F32)
        pD = psum("pD", [128, 512], F32)
        pE = psum("pE", [128, 512], F32)
        pF = psum("pF", [128, 512], F32)
        pG = psum("pG", [128, 512], F32)
        pH = psum("pH", [128, 512], F32)
        pGb = pG[:].bitcast(BF16).rearrange("p (k t) -> p k t", k=8)
        pHb = pH[:].bitcast(BF16).rearrange("p (k t) -> p k t", k=8)

        S.dma("pool", win[:], win_d.rearrange("(k p) n -> p k n", p=128), [], ["win"], "win")
        S.dma("pool", wout[:], wout_d.rearrange("(k p) n -> p k n", p=128), [], ["wout"], "wout")
        S.dma("pool", msk[:], masks_d[:, :, :], [], ["msk"], "msk")
        S.dma("sp", trilf[:], masks_d[:, 3, 0:128], [], ["trilf"], "c0")
        S.dma("sp", g1b[:], g1_d[0:1, :].partition_broadcast(128), [], ["g1b"], "c1")
        S.dma("sp", gmb[:, 0:512], ga_d[0:1, :].partition_broadcast(128), [], ["gmb0"], "c2")
        S.dma("sp", gmb[:, 512:1024], gg_d[0:1, :].partition_broadcast(128), [], ["gmb1"], "c3")
        S.dma("sp", g2b[:], g2_d[0:1, :].partition_broadcast(128), [], ["g2b"], "c4")
        S.dma("sp", gfb[:], gf_d[0:1, :].partition_broadcast(128), [], ["gfb"], "c5")
        S.dma("sp", lngb[:], lng_d[0:1, :].partition_broadcast(128), [], ["lngb"], "c6")
        S.dma("sp", lnbb[:], lnb_d[0:1, :].partition_broadcast(128), [], ["lnbb"], "c7")
        S.dma("sp", bsb[:], bsp_d.rearrange("h t -> t h"), [], ["bsb"], "c8", ncok=True)
        S.dma("sp", esink[:], sinks_d[0:1, :].partition_broadcast(128), [], ["esink"], "c9")
        S.dma("sp", wsf, wsp_d.rearrange("h t c -> t h c"), [], ["hn2@0"], "c10")
        S.dma("sp", wr[:, :, 0:4], wgr_d.rearrange("(p k) n -> p k n", k=8), [], ["wr0"], "c11")
        for g in range(4):
            S.dma("sp", wr[:, :, 4 + 8 * g:12 + 8 * g], wer_d[g].rearrange("(p k) n -> p k n", k=8), [], [f"wr{g + 1}"], f"c{12 + g}")
        S.dma("sp", rbias[:, 0:4], bgr_d[0:1, :].partition_broadcast(128), [], ["rb0"], "c16")
        S.dma("sp", rbias[:, 4:36], ber_d[0:1, :].partition_broadcast(128), [], ["rb1"], "c17")
        WR = ["wr0", "wr1", "wr2", "wr3", "wr4"]
        S.dma("sp", tokid[:], tokid_d[:, :], [], ["tokid"], "c18")
        S.dma("sp", dstt[:], dst_d[:, :, :], [], ["dstt"], "c20")
        S.dma("sp", ecap[:], ecap_d[0:1, :].partition_broadcast(128), [], ["ecap"], "c19")
        S.add("pool", lambda: nc.gpsimd.memset(onesb[:], 1.0), [], ["onesb"])
        S.add("pool", lambda: nc.gpsimd.memset(utri[:], 1.0), [], ["utri"])
        S.add("pool", lambda: nc.gpsimd.affine_select(out=utri[:], in_=utri[:], pattern=[[1, 128]], compare_op=ALU.is_gt,
                                                      fill=0.0, base=0, channel_multiplier=-1), ["utri"], ["utri"])

        S.add("pool", lambda: nc.gpsimd.memset(S.dummy[:], 1.0), [], ["dummy"])
        S.add("pool", lambda: nc.gpsimd.memset(identf[:], 0.0), [], ["identf"])
        S.add("pool", lambda: nc.gpsimd.affine_select(out=identf[:], in_=identf[:], pattern=[[-1, 128]], compare_op=ALU.not_equal,
                                                      fill=1.0, base=0, channel_multiplier=1), ["identf"], ["identf"])
        S.copy("dve", identb[:], identf[:], ["identf"], ["identb"])
        S.add("pool", lambda: nc.gpsimd.memset(vaug[:], 1.0), [], ["vaug0", "vaug1", "vaug2"])
        S.actv(esink[:], esink[:], AF.Exp, ["esink"], ["esink"])
        S.tt("dve", wsm, wsf, trilf[:].unsqueeze(1).to_broadcast([128, 8, 128]), ALU.mult, ["hn2@0", "trilf"], ["mixed0@0", "mixed1@0"])
        S.tr([(pA[:, h, :], wsm[:, h, :], identb[:]) for h in range(8)], ["mixed0@0", "mixed1@0", "identb"], ["pA"])
        S.copy("dve", wsT[:], pA, ["pA"], ["wsT"])

        def rsqrt_chain(y, a, t, key, n_iter=2):
            S.actv(t, a, AF.Ln, [key + "a"], [key + "t"])
            S.actv(y, t, AF.Exp, [key + "t"], [key + "y"], scale=-0.5)

        def block(gb, halo=False):
            b = gb % BPP
            par = gb % 2
            S.suffix = "@%d" % par
            junk, hn, hnT, qT, u_t, vg_t, vn_t = JUNK[par], HN[par], HNT[par], QT[par], UT[par], VGT[par], VNT[par]
            pt, a_t, gm_t, mixed, mixedT, hn2, hn2T32, st = PT[par], AT[par], GMT[par], MIXED[par], MIXEDT[par], HN2[par], HN2T32[par], STT[par]
            slot = gb % 3
            pslot = (gb - 1) % 3
            xs = xt[gb % 2]
            xk = f"xt{gb % 2}"
            if halo:
                S.dma("sp", xs[:], xh_d[:, :], [], [xk], xk)
            else:
                S.dma("sp", xs[:], x_d[gb * 128:(gb + 1) * 128, :], [], [xk], xk)
            S.actv(junk[:], xs[:], AF.Square, [xk], ["junk", "st0"], scale=1.0 / 32, accum=st[:, 0:1])
            S.ts("dve", st[:, 1:2], st[:, 0:1], RS_EPS, None, ALU.add, None, ["st0"], ["r1a"])
            rsqrt_chain(st[:, 2:3], st[:, 1:2], st[:, 3:4], "r1")
            S.stt("dve", hn[:], xs[:], st[:, 2:3], g1b[:], ALU.mult, ALU.mult, [xk, "r1y", "g1b"], ["hn"])
            yield
            S.suffix = "@%d" % par
            S.tr([(pA[:, k, :], hn[:, k * 128:(k + 1) * 128], identb[:]) for k in range(8)], ["hn", "identb"], ["pA"])
            S.copy("act", hnT[:], pA, ["pA"], ["hnT"])
            yield
            S.suffix = "@%d" % par
            lst = []
            if not halo:
                for c in range(4):
                    for k in range(8):
                        lst.append((pB[:, c * 128:(c + 1) * 128], win[:, k, c * 128:(c + 1) * 128], hnT[:, k, :], k == 0, k == 7))
            for k in range(8):
                lst.append((pC[:, 0:128], win[:, k, 512:640], hnT[:, k, :], k == 0, k == 7))
            S.mm(lst, ["win", "hnT"], ["pB", "pC"])
            lst = []
            for k in range(8):
                lst.append((pF[:, 0:128], hnT[:, k, :], win[:, k, 640:768], k == 0, k == 7))
            if not halo:
                for k in range(8):
                    lst.append((pD[:, :], hnT[:, k, :], win[:, k, 768:1280], k == 0, k == 7))
                for k in range(8):
                    lst.append((pE[:, :], hnT[:, k, :], win[:, k, 1280:1792], k == 0, k == 7))
            S.mm(lst, ["win", "hnT"], ["pD", "pE", "pF"])
            S.copy("act", kT[:, slot, :], pC[:, 0:128], ["pC"], [f"kT{slot}"])
            S.copy("dve", vaug[:, slot, :, 0:64], pF[:, 0:128].rearrange("p (g d) -> p g d", g=2), ["pF"], [f"vaug{slot}"])
            if halo:
                return
            S.copy("dve", qT[:], pB[:].rearrange("p (c t) -> p c t", c=4), ["pB"], ["qT"])
            S.actv(u_t[:], pD[:], AF.Gelu, ["pD"], ["u_t"])
            S.actv(vg_t[:], pE[:], AF.Gelu, ["pE"], ["vg_t", "st8"], accum=st[:, 8:9])
            S.actv(junk[:, 0:512], vg_t[:], AF.Square, ["vg_t"], ["junk", "st9"], accum=st[:, 9:10])
            yield
            S.suffix = "@%d" % par
            for g in range(2):
                for kb in range(2):
                    ks = pslot if kb == 0 else slot
                    pp = pG if kb == 0 else pH
                    pk = "pG" if kb == 0 else "pH"
                    S.mm([(pp[:, :], kT[g * 64:(g + 1) * 64, ks, :], qT[g * 64:(g + 1) * 64, :, :], True, True)],
                         [f"kT{ks}", "qT"], [pk])
                    S.actv(pt[g][kb][:], pp[:].rearrange("p (c t) -> p c t", c=4), AF.Exp, [pk], [f"p{g}{kb}"], scale=0.125)
                    mi = (2 if gb == 0 else 1) if kb == 0 else 0
                    S.tt("pool", pt[g][kb][:], pt[g][kb][:], msk[:, mi, :].rearrange("p (c t) -> p c t", c=4), ALU.mult,
                         [f"p{g}{kb}", "msk"], [f"p{g}{kb}"])
            yield
            S.suffix = "@%d" % par
            for g in range(2):
                pv = pB if g == 0 else pC
                lst = []
                for c in range(4):
                    for kb in range(2):
                        ks = pslot if kb == 0 else slot
                        lst.append((pv[:, c * 65:(c + 1) * 65], pt[g][kb][:, c, :], vaug[:, ks, g, :], kb == 0, kb == 1))
                S.mm(lst, [f"p{g}0", f"p{g}1", f"vaug{pslot}", f"vaug{slot}"], ["pB" if g == 0 else "pC"])
            for g in range(2):
                pv = pB if g == 0 else pC
                pvk = "pB" if g == 0 else "pC"
                pv3 = pv[:, 0:260].rearrange("p (c e) -> p c e", c=4)
                S.tt("dve", st[:, 16 + 4 * g:20 + 4 * g], pv3[:, :, 64], esink[:, 4 * g:4 * g + 4], ALU.add, [pvk, "esink"], [f"den{g}"])
                S.add("dve", (lambda g=g: nc.vector.reciprocal(out=st[:, 24 + 4 * g:28 + 4 * g], in_=st[:, 16 + 4 * g:20 + 4 * g])),
                      [f"den{g}"], [f"rden{g}"])
                S.tt("dve", a_t[:, 4 * g:4 * g + 4, :], pv3[:, :, 0:64],
                     st[:, 24 + 4 * g:28 + 4 * g].unsqueeze(2).to_broadcast([128, 4, 64]), ALU.mult, [pvk, f"rden{g}"], [f"a_t{g}"])
            S.actv(junk[:, 0:512], a_t[:].rearrange("p h d -> p (h d)"), AF.Square, ["a_t0", "a_t1"], ["junk", "st10"], accum=st[:, 10:11])
            yield
            S.suffix = "@%d" % par
            S.ts("dve", st[:, 32:33], st[:, 8:9], 1.0 / 512, None, ALU.mult, None, ["st8"], ["lnm"])
            S.tt("dve", st[:, 33:34], st[:, 32:33], st[:, 32:33], ALU.mult, ["lnm"], ["lnm2"])
            S.stt("dve", st[:, 34:35], st[:, 9:10], 1.0 / 512, st[:, 33:34], ALU.mult, ALU.subtract, ["st9", "lnm2"], ["lnv"])
            S.ts("dve", st[:, 35:36], st[:, 34:35], LN_EPS, None, ALU.add, None, ["lnv"], ["r2a"])
            rsqrt_chain(st[:, 36:37], st[:, 35:36], st[:, 37:38], "r2")
            S.ts("dve", vg_t[:], vg_t[:], st[:, 32:33], st[:, 36:37], ALU.subtract, ALU.mult, ["vg_t", "lnm", "r2y"], ["vg_t"])
            S.tt("dve", vg_t[:], vg_t[:], lngb[:], ALU.mult, ["vg_t", "lngb"], ["vg_t"])
            S.tt("dve", vn_t[:], vg_t[:], lnbb[:], ALU.add, ["vg_t", "lnbb"], ["vn_t"])
            S.mm([(pD[:, h * 64:(h + 1) * 64], wsT[:, h, :], vn_t[:, h * 64:(h + 1) * 64], True, True) for h in range(8)],
                 ["wsT", "vn_t"], ["pD"])
            S.tt("dve", gm_t[:], pD[:].rearrange("p (h d) -> p h d", h=8), bsb[:].unsqueeze(2).to_broadcast([128, 8, 64]), ALU.add,
                 ["pD", "bsb"], ["gm_t"])
            gmf = gm_t[:].rearrange("p h d -> p (h d)")
            S.tt("dve", gmf, gmf, u_t[:], ALU.mult, ["gm_t", "u_t"], ["gm_t"])
            S.actv(junk[:, 0:512], gmf, AF.Square, ["gm_t"], ["junk", "st11"], accum=st[:, 11:12])
            yield
            S.suffix = "@%d" % par
            S.ts("dve", st[:, 40:42], st[:, 10:12], 1.0 / 512, RS_EPS, ALU.mult, ALU.add, ["st10", "st11"], ["r3a"])
            rsqrt_chain(st[:, 42:44], st[:, 40:42], st[:, 44:46], "r3")
            S.stt("dve", mixed[:, 0:512], a_t[:].rearrange("p h d -> p (h d)"), st[:, 42:43], gmb[:, 0:512], ALU.mult, ALU.mult,
                  ["a_t0", "a_t1", "r3y", "gmb0"], ["mixed0"])
            S.stt("dve", mixed[:, 512:1024], gmf, st[:, 43:44], gmb[:, 512:1024], ALU.mult, ALU.mult,
                  ["gm_t", "r3y", "gmb1"], ["mixed1"])
            S.tr([(pGb[:, k, :], mixed[:, k * 128:(k + 1) * 128], identb[:]) for k in range(8)], ["mixed0", "mixed1", "identb"], ["pG"])
            S.copy("act", mixedT[:], pGb, ["pG"], ["mixedT"])
            yield
            S.suffix = "@%d" % par
            lst = []
            for n, pp in enumerate((pG, pH)):
                for k in range(8):
                    lst.append((pp[:, :], mixedT[:, k, :], wout[:, k, n * 512:(n + 1) * 512], k == 0, k == 7))
            S.mm(lst, ["mixedT", "wout"], ["pG", "pH"])
            h1s = h1t[gb % 2]
            h1k = f"h1t{gb % 2}"
            S.tt("dve", h1s[:, 0:512], pG[:], xs[:, 0:512], ALU.add, ["pG", xk], [h1k + "a"])
            S.tt("dve", h1s[:, 512:1024], pH[:], xs[:, 512:1024], ALU.add, ["pH", xk], [h1k + "b"])
            S.dma("sp", h1_d[gb * 128:(gb + 1) * 128, :], h1s[:], [h1k + "a", h1k + "b"], [f"h1d_{gb}"], f"h1st{gb % 2}")
            S.actv(junk[:], h1s[:], AF.Square, [h1k + "a", h1k + "b"], ["junk", "st12"], scale=1.0 / 32, accum=st[:, 12:13])
            S.ts("dve", st[:, 48:49], st[:, 12:13], RS_EPS, None, ALU.add, None, ["st12"], ["r4a"])
            rsqrt_chain(st[:, 49:50], st[:, 48:49], st[:, 50:51], "r4")
            S.stt("dve", hn2[:], h1s[:], st[:, 49:50], g2b[:], ALU.mult, ALU.mult, [h1k + "a", h1k + "b", "r4y", "g2b"], ["hn2"])
            S.dma("pool", hn2_d[gb * 128:(gb + 1) * 128, :], hn2[:], ["hn2"], [f"hn2d_{gb}"], f"hn2st{par}")
            yield
            S.suffix = "@%d" % par
            pDE = [pD, pD, pD, pD, pE, pE, pE, pE]
            S.tr([(pDE[k][:, (k % 4) * 128:(k % 4 + 1) * 128], hn2[:, bass.ds(k, 128, step=8)], identf[:]) for k in range(8)],
                 ["hn2", "identf"], ["pD", "pE"])
            S.copy("act", hn2T32[:, 0:4, :], pD[:].rearrange("p (k t) -> p k t", k=4), ["pD"], ["hn2T32a"])
            S.copy("act", hn2T32[:, 4:8, :], pE[:].rearrange("p (k t) -> p k t", k=4), ["pE"], ["hn2T32b"])
            S.mm([(pF[:, 0:36], hn2T32[:, k, :], wr[:, k, :], k == 0, k == 7) for k in range(8)], ["hn2T32a", "hn2T32b"] + WR, ["pF"])
            S.tt("dve", lg[:, b, :], pF[:, 0:36], rbias[:], ALU.add, ["pF", "rb0", "rb1"], [f"lg_{b}"])

        def routing():
            LG = [f"lg_{b}" for b in range(BPP)]
            gl = lg[:, :, 0:4]
            el = lg[:, :, 4:36].rearrange("p b (j e) -> p b j e", j=4)
            gmax = rt[:, :, 0:1]
            ohg = rt[:, :, 1:5]
            gex = rt[:, :, 5:9]
            gsum = rt[:, :, 9:10]
            gw = rt[:, :, 10:11]
            tmp = rt[:, :, 16:48].rearrange("p b (j e) -> p b j e", j=4)
            esel = rt[:, :, 48:56]
            m1 = rt[:, :, 11:12]
            m2 = rt[:, :, 12:13]
            eq1 = rt[:, :, 56:64]
            e2 = rt[:, :, 64:72]
            eq2 = rt[:, :, 72:80]
            dd = rt[:, :, 13:14]
            w1 = rt[:, :, 14:15]
            w2 = rt[:, :, 15:16]
            wn = rt[:, :, 80:88]
            S.red("dve", rt[:, :, 0], gl, ALU.max, LG, ["gmax"])
            S.tt("dve", ohg, gl, gmax.to_broadcast([128, BPP, 4]), ALU.is_equal, LG + ["gmax"], ["ohg"])
            S.tt("dve", gex, gl, gmax.to_broadcast([128, BPP, 4]), ALU.subtract, LG + ["gmax"], ["gex"])
            S.actv(gex, gex, AF.Exp, ["gex"], ["gex"])
            S.red("dve", rt[:, :, 9], gex, ALU.add, ["gex"], ["gsum"])
            S.add("dve", lambda: nc.vector.reciprocal(out=gw, in_=gsum), ["gsum"], ["gw"])
            S.tt("dve", tmp, el, ohg.unsqueeze(3).to_broadcast([128, BPP, 4, 8]), ALU.mult, LG + ["ohg"], ["tmp"])
            S.red("dve", esel, tmp.rearrange("p b j e -> p b e j"), ALU.add, ["tmp"], ["esel"])
            S.red("dve", rt[:, :, 11], esel, ALU.max, ["esel"], ["m1"])
            S.tt("dve", eq1, esel, m1.to_broadcast([128, BPP, 8]), ALU.is_equal, ["esel", "m1"], ["eq1"])
            S.stt("dve", e2, eq1, -1e30, esel, ALU.mult, ALU.add, ["eq1", "esel"], ["e2"])
            S.red("dve", rt[:, :, 12], e2, ALU.max, ["e2"], ["m2"])
            S.tt("dve", eq2, e2, m2.to_broadcast([128, BPP, 8]), ALU.is_equal, ["e2", "m2"], ["eq2"])
            S.tt("dve", dd, m2, m1, ALU.subtract, ["m1", "m2"], ["dd"])
            S.actv(dd, dd, AF.Exp, ["dd"], ["dd"])
            S.ts("dve", w1, dd, 1.0, None, ALU.add, None, ["dd"], ["w1"])
            S.add("dve", lambda: nc.vector.reciprocal(out=w1, in_=w1), ["w1"], ["w1"])
            S.tt("dve", w2, dd, w1, ALU.mult, ["dd", "w1"], ["w2"])
            S.tt("dve", w1, w1, gw, ALU.mult, ["w1", "gw"], ["w1"])
            S.tt("dve", w2, w2, gw, ALU.mult, ["w2", "gw"], ["w2"])
            S.tt("dve", eq1, eq1, w1.to_broadcast([128, BPP, 8]), ALU.mult, ["eq1", "w1"], ["eq1"])
            S.tt("dve", eq2, eq2, w2.to_broadcast([128, BPP, 8]), ALU.mult, ["eq2", "w2"], ["eq2"])
            S.tt("dve", wn, eq1, eq2, ALU.add, ["eq1", "eq2"], ["wn"])
            for j in range(4):
                S.tt("dve", gates[:, :, j * 8:(j + 1) * 8], wn, ohg[:, :, j:j + 1].to_broadcast([128, BPP, 8]), ALU.mult,
                     ["wn", "ohg"], ["gates"])

        def compaction():
            Mf = rt[:, :, 16:48]
            S.add("dve", lambda: nc.vector.tensor_single_scalar(out=Mf, in_=gates[:], scalar=0.0, op=ALU.is_gt), ["gates", "tmp"], ["tmp"])
            S.copy("dve", Mb[:], Mf, ["tmp"], ["Mb"])
            S.add("pool", lambda: nc.gpsimd.memset(Macc[:, 0, :], 0.0), [], ["Macc"])
            for b in range(BPP):
                S.tt("dve", Macc[:, b + 1, :], Macc[:, b, :], Mb[:, b, :], ALU.add, ["Macc", "Mb"], ["Macc"])
            for b in range(BPP):
                bank, bk = (pB, "pB") if (b // 16) % 2 == 0 else (pC, "pC")
                o = bank[:, (b % 16) * 32:(b % 16 + 1) * 32]
                S.mm([(o, utri[:], Mb[:, b, :], True, False), (o, onesb[:], Macc[:, b, :], False, True)], ["utri", "onesb", "Mb", "Macc"], [bk])
                if b % 16 == 15 or b == BPP - 1:
                    b0 = (b // 16) * 16
                    nb = b - b0 + 1
                    S.tt("dve", slf[:, b0:b0 + nb, :], bank[:, 0:nb * 32].rearrange("p (b e) -> p b e", e=32),
                         ecap[:].unsqueeze(1).to_broadcast([128, nb, 32]), ALU.add, [bk, "ecap"], [f"slf{b0}"])
            SLF = [f"slf{b0}" for b0 in range(0, BPP, 16)]
            S.mm([(pD[:, 0:32], onesb[:], Macc[:, BPP, :], True, True)], ["onesb", "Macc"], ["pD"])
            S.copy("dve", cnt_i[:], pD[:, 0:32], ["pD"], ["cnt_i"])
            sel = rt[:, :, 48:80]
            S.stt("dve", sel, slf[:], 1.0, Mf, ALU.add, ALU.mult, SLF + ["tmp"], ["sel"])
            S.ts("dve", sel, sel, -1.0, None, ALU.add, None, ["sel"], ["sel"])
            sA = rt[:, :, 0]
            sB = rt[:, :, 1]
            eqm = rt[:, :, 16:48]
            S.red("dve", sA, sel, ALU.max, ["sel"], ["sA"])
            S.tt("dve", eqm, sel, rt[:, :, 0:1].to_broadcast([128, BPP, 32]), ALU.is_equal, ["sel", "sA", "tmp"], ["tmp"])
            S.tt("dve", slf[:], eqm, gates[:], ALU.mult, ["tmp", "gates"] + SLF, SLF)
            S.red("dve", infoA[:, :, 2], slf[:], ALU.add, SLF, ["infoA1"])
            S.stt("dve", sel, eqm, -1e9, sel, ALU.mult, ALU.add, ["tmp", "sel"], ["sel"])
            S.red("dve", sB, sel, ALU.max, ["sel"], ["sB"])
            S.tt("dve", eqm, sel, rt[:, :, 1:2].to_broadcast([128, BPP, 32]), ALU.is_equal, ["sel", "sB", "tmp"], ["tmp"])
            S.tt("dve", slf[:], eqm, gates[:], ALU.mult, ["tmp", "gates"] + SLF, SLF)
            S.red("dve", infoB[:, :, 2], slf[:], ALU.add, SLF, ["infoB1"])
            S.copy("dve", infoA[:].bitcast(I32)[:, :, 0], tokid[:], ["tokid"], ["infoA0"])
            S.copy("dve", infoB[:].bitcast(I32)[:, :, 0], tokid[:], ["tokid"], ["infoB0"])
            S.copy("dve", infoA[:].bitcast(I32)[:, :, 1], dstt[:, :, 0], ["dstt"], ["infoA0"])
            S.copy("dve", infoB[:].bitcast(I32)[:, :, 1], dstt[:, :, 1], ["dstt"], ["infoB0"])
            S.add("pool", lambda: nc.gpsimd.memset(infoA[:, :, 3], 0.0), [], ["infoA0"])
            S.add("pool", lambda: nc.gpsimd.memset(infoB[:, :, 3], 0.0), [], ["infoB0"])
            S.copy("dve", sAi[:], sA, ["sA"], ["sAi"])
            S.copy("dve", sBi[:], sB, ["sB"], ["sBi"])
            zt = junk[:].bitcast(F32)
            S.add("pool", lambda: nc.gpsimd.memset(zt, 0.0), ["junk@0"], ["junk@0"])
            S.add("pool", lambda: nc.gpsimd.memset(zt.bitcast(I32).rearrange("p (r c) -> p r c", c=4)[:, :, 0], TOK), ["junk@0"], ["junk@0"])
            S.add("pool", lambda: nc.gpsimd.memset(zt.bitcast(I32).rearrange("p (r c) -> p r c", c=4)[:, :, 1], 2 * TOK), ["junk@0"], ["junk@0"])
            iv = info_d.rearrange("(p r) c -> p (r c)", p=128)
            ncol = NE * CAP * 4 // 128
            zw = min(512, ncol)
            for c0 in range(0, ncol, zw):
                S.dma("sp", iv[:, c0:c0 + zw], zt.bitcast(I32)[:, 0:zw], ["junk@0"], ["info"], f"iz{c0 // zw % 8}")
            for b in range(BPP):
                S.add("pool", (lambda b=b: nc.gpsimd.indirect_dma_start(
                    out=info_d[:, :], out_offset=bass.IndirectOffsetOnAxis(ap=sAi[:, b:b + 1], axis=0), in_=infoA[:].bitcast(I32)[:, b, :], in_offset=None)),
                    ["sAi", "infoA0", "infoA1", "info"], [f"infoA_{b}"], dma="scat")
                S.add("pool", (lambda b=b: nc.gpsimd.indirect_dma_start(
                    out=info_d[:, :], out_offset=bass.IndirectOffsetOnAxis(ap=sBi[:, b:b + 1], axis=0), in_=infoB[:].bitcast(I32)[:, b, :], in_offset=None)),
                    ["sBi", "infoB0", "infoB1", "info"], [f"infoB_{b}"], dma="scat")

        H1ALL = [f"h1d_{gb}" for gb in range(NBLK)]
        INFO_ALL = [f"infoA_{b}" for b in range(BPP)] + [f"infoB_{b}" for b in range(BPP)]
        HN2D = [f"hn2d_{gb}" for gb in range(NBLK)]

        def convert_expert(e):
            S.dma("pool", wgb_d[e], wg_d[e], [], [f"wcv_{e}g"], f"wcv{e % 4}")
            S.dma("pool", wub_d[e], wu_d[e], [], [f"wcv_{e}u"], f"wcv{e % 4}")
            S.dma("pool", wdb_d[e], wd_d[e], [], [f"wcv_{e}d"], f"wcv{e % 4}")

        def load_expert(e):
            s = e % 2
            WCV = [f"wcv_{ee}{t}" for ee in range(e % 4, NE, 4) for t in "gud"]
            S.dma("sp", wgt[s][:], wgb_d[e].rearrange("(p k) n -> p k n", k=8), WCV, [f"wgt{s}"], f"wgt{s}")
            S.dma("sp", wup[s][:], wub_d[e].rearrange("(p k) n -> p k n", k=8), WCV, [f"wup{s}"], f"wup{s}")
            S.dma("sp", wdn[s][:], wdb_d[e].rearrange("(m p) n -> p m n", p=128), WCV, [f"wdn{s}"], f"wdn{s}")

        tile_it = [0]

        bc_reg = nc.gpsimd.alloc_register("bc")
        nc.gpsimd.reg_mov(bc_reg, TOK - 1)
        bc2_reg = nc.gpsimd.alloc_register("bc2")
        nc.gpsimd.reg_mov(bc2_reg, 2 * TOK - 1)

        tile_q = {}
        J1_INLINE = min(3, JMAX)

        def tile_A(e, j):
            for _ in tile_A_gen(e, j):
                pass

        def tile_A_gen(e, j):
            s = e % 2
            q = j % 2
            if j < J1_INLINE:
                qx = j
                qb = j if j < 2 else 4
            else:
                qx = q
                qb = 2 + q
            qs = qb if (j >= J1_INLINE or e % 2 == 0) else (5 + j)
            tile_q[(e, j)] = (qb, qs)
            r0 = e * CAP + j * 128
            S.dma("sp", si[qs][:], info_d[r0:r0 + 128, :], INFO_ALL, [f"si{qs}"], f"si{qs}")
            S.add("pool", (lambda qx=qx, qb=qb: nc.gpsimd.indirect_dma_start(
                out=xg[qx], out_offset=None, in_=hn2_d[:, :], in_offset=bass.IndirectOffsetOnAxis(ap=si[qs][:, 0:1], axis=0),
                bounds_check=bc_reg, oob_is_err=False)),
                [f"si{qs}"] + HN2D, [f"xg{qx}"], dma=f"xg{qx}")
            yield
            ptr, ptrk = (pA, "pA") if q == 0 else (pHb, "pH")
            S.tr([(ptr[:, k, :], xg[qx][:, bass.ds(k, 128, step=8)], identb[:]) for k in range(8)], [f"xg{qx}", "identb"], [ptrk])
            yield
            S.copy("act", xgT[qx], ptr, [ptrk], [f"xgT{qx}"])
            yield
            pgu, pguk = (pB, "pB") if q == 0 else (pC, "pC")
            lst = []
            for m in range(2):
                for k in range(8):
                    lst.append((pgu[:, m * 128:(m + 1) * 128], wgt[s][:, k, m * 128:(m + 1) * 128], xgT[qx][:, k, :], k == 0, k == 7))
            for m in range(2):
                for k in range(8):
                    lst.append((pgu[:, 256 + m * 128:256 + (m + 1) * 128], wup[s][:, k, m * 128:(m + 1) * 128], xgT[qx][:, k, :], k == 0, k == 7))
            S.mm(lst, [f"wgt{s}", f"wup{s}", f"xgT{qx}"], [pguk])
            yield
            S.actv(sil[qx][:], pgu[:, 0:256], AF.Silu, [pguk], [f"sil{qx}"])
            S.tt("dve", hid[qx][:].rearrange("p m t -> p (m t)"), pgu[:, 256:512], sil[qx][:], ALU.mult, [pguk, f"sil{qx}"], [f"hid{qx}"])
            yield
            pd = (pD, pE) if q == 0 else (pF, pG)
            pdk = ("pD", "pE") if q == 0 else ("pF", "pG")
            lst = []
            for n in range(2):
                for m in range(2):
                    lst.append((pd[n][:, :], hid[qx][:, m, :], wdn[s][:, m, n * 512:(n + 1) * 512], m == 0, m == 1))
            S.mm(lst, [f"hid{qx}", f"wdn{s}"], list(pdk))
            yield
            S.actv(yt[qb][:, 0:512], pd[0][:], AF.Copy, [pdk[0], f"si{qs}"], [f"yt{qb}a"], scale=si[qs][:, 2:3].bitcast(F32))
            S.ts("dve", yt[qb][:, 512:1024], pd[1][:], si[qs][:, 2:3].bitcast(F32), None, ALU.mult, None, [pdk[1], f"si{qs}"], [f"yt{qb}b"])

        b_cnt = {}

        def tile_B(e, j):
            q, qs = tile_q[(e, j)]
            c = b_cnt.get((e, j), 0)
            b_cnt[(e, j)] = c + 1
            S.add("pool", (lambda q=q, qs=qs: nc.gpsimd.indirect_dma_start(
                out=y2_d[:, :], out_offset=bass.IndirectOffsetOnAxis(ap=si[qs][:, 1:2], axis=0), in_=yt[q], in_offset=None,
                bounds_check=bc2_reg, oob_is_err=False)),
                [f"yt{q}a", f"yt{q}b", f"si{qs}"], [f"y2_{e}_{j}_{c}"], dma=f"yst{q}")
            Y2ALL.append(f"y2_{e}_{j}_{c}")

        Y2ALL = []
        EMAP = {mybir.EngineType.Activation: "act", mybir.EngineType.DVE: "dve", mybir.EngineType.PE: "pe",
                mybir.EngineType.Pool: "pool", mybir.EngineType.SP: "sp"}

        def moe_sparse():
            regs = nc.alloc_registers("n_e")
            RS = [regs, regs]
            NUNC = min(3, JMAX)
            J1 = J1_INLINE

            def cond(e, j, fn):
                if j < NUNC:
                    fn(e, j)
                else:
                    S.begin_if(j * 128, RS[e % 2])
                    fn(e, j)
                    S.end_if()

            def tail(e, j=None):
                if j is None:
                    j = J1
                if j >= JMAX:
                    return
                S.begin_if(j * 128, regs)
                tile_A(e, j)
                if j > J1:
                    tile_B(e, j - 1)
                if j + 1 < JMAX:
                    tail(e, j + 1)
                    S.begin_if((j + 1) * 128, regs, invert=True)
                    tile_B(e, j)
                    S.end_if()
                else:
                    tile_B(e, j)
                S.end_if()

            assert NUNC == J1
            def run_gens(gens):
                hold = [0, 0, 2][:len(gens)]
                while gens:
                    alive = []
                    for gi, g in enumerate(gens):
                        if hold[gi] > 0:
                            hold[gi] -= 1
                            alive.append((g, hold[gi]))
                            continue
                        try:
                            next(g)
                            alive.append((g, 0))
                        except StopIteration:
                            pass
                    gens = [g for g, _ in alive]
                    hold = [h for _, h in alive]

            def front(e):
                gens = [tile_A_gen(e, jj) for jj in range(NUNC)]
                for g in gens:
                    next(g)
                return gens

            gens = front(0)
            for e in range(NE_RUN):
                for r in regs:
                    S.add(EMAP[r.engine], (lambda r=r, e=e: nc.reg_load(r, cnt_i[0:1, e:e + 1])), ["cnt_i"], [])
                if e + 1 < NE_RUN:
                    load_expert(e + 1)
                run_gens(gens)
                tail(e)
                if e + 1 < NE_RUN:
                    gens = front(e + 1)
                for jj in range(J1):
                    tile_B(e, jj)

        NCL = 3

        def combine_block(b):
            q = b % NCL
            S.dma("sp", hc[q], h1_d[b * 128:(b + 1) * 128, :], H1ALL + Y2ALL, [f"hc{q}"], f"hc{q}")
            S.dma("sp", yc[q], y2_d[2 * b * 128:2 * (b + 1) * 128, :].rearrange("(p k) d -> p k d", k=2), Y2ALL, [f"yc{q}"], f"yc{q}")
            yield
            S.tt("pool", yc[q][:, 0, :], yc[q][:, 0, :], yc[q][:, 1, :], ALU.add, [f"yc{q}"], [f"yc{q}"])
            yield
            S.tt("dve", hc[q], hc[q], yc[q][:, 0, :], ALU.add, [f"hc{q}", f"yc{q}"], [f"hc{q}"])
            yield
            k0 = 40 + 4 * q
            S.actv(junk[:], hc[q], AF.Square, [f"hc{q}"], ["junk@0", f"f{q}s"], scale=1.0 / 32, accum=st[:, k0:k0 + 1])
            yield
            S.ts("dve", st[:, k0 + 1:k0 + 2], st[:, k0:k0 + 1], RS_EPS, None, ALU.add, None, [f"f{q}s"], [f"f{q}a"])
            rsqrt_chain(st[:, k0 + 2:k0 + 3], st[:, k0 + 1:k0 + 2], st[:, k0 + 3:k0 + 4], f"f{q}")
            S.stt("dve", hc[q], hc[q], st[:, k0 + 2:k0 + 3], gfb[:], ALU.mult, ALU.mult, [f"hc{q}", f"f{q}y", "gfb"], [f"hc{q}"])
            yield
            S.dma("sp", out_d[b * 128:(b + 1) * 128, :], hc[q], [f"hc{q}"], [], f"ost{q}")

        def combine():
            def clane(start):
                for b in range(start, NBLK, NCL):
                    yield from combine_block(b)
            lanes = [clane(i) for i in range(NCL)]
            for i in range(NCL):
                for _ in range(2 * (NCL - 1 - i)):
                    next(lanes[i])
            while lanes:
                for ln in list(lanes):
                    try:
                        next(ln)
                    except StopIteration:
                        lanes.remove(ln)

        for _ in block(-1, halo=True):
            pass

        def lane(start):
            for gb in range(start, NBLK, 2):
                if gb < NE:
                    sv = S.suffix
                    S.suffix = ""
                    convert_expert(gb)
                    S.suffix = sv
                yield from block(gb)

        lanes = [lane(0), lane(1)]
        for _ in range(5):
            next(lanes[0])
        while lanes:
            for ln in list(lanes):
                try:
                    next(ln)
                except StopIteration:
                    lanes.remove(ln)
        S.suffix = ""
        for e in range(NBLK, NE):
            convert_expert(e)
        routing()
        compaction()
        S.add("pool", lambda: nc.gpsimd.memset(st[:, 60:61], 0.0), ["win", "wout"],
              ["win", "wout", "xg0", "xg1", "xgT0", "xgT1", "yt0a", "yt0b", "yt1a", "yt1b", "yt2a", "yt2b", "yt3a", "yt3b", "yt4a", "yt4b", "xg2", "xgT2", "yc0", "yc1", "yc2", "hc0", "hc1", "hc2"])
        S.add("pool", lambda: nc.gpsimd.memset(woutf[:, 0:2048], 0.0), ["xg0", "xg1"], ["xg0", "xg1"])
        S.add("pool", lambda: nc.gpsimd.memset(xg[2], 0.0), ["xg2"], ["xg2"])
        load_expert(0)
        moe_sparse()
        combine()
        S.emit()
        globals()['_LAST_NDUMMY'] = S.ndummy
    return nc


_NC_CACHE = {}


def _prep_inputs(inputs):
    f = lambda a: np.ascontiguousarray(np.asarray(a, dtype=np.float32))
    x = f(inputs["x"])
    B, Sq, _ = x.shape
    w_in = f(inputs["w_in"])[0]
    perm = []
    for c in range(4):
        perm += list(range(c * 64, c * 64 + 64)) + list(range((4 + c) * 64, (4 + c) * 64 + 64))
    perm += list(range(512, INP))
    w_in_p = np.ascontiguousarray(w_in[:, perm])
    s_idx = np.arange(128)[:, None]
    q_idx = np.arange(128)[None, :]
    mcur = (s_idx <= q_idx).astype(np.float32)
    mprev = (s_idx > q_idx).astype(np.float32)
    tril = (q_idx <= s_idx).astype(np.float32)
    common = {
        "mix_norm_g": f(inputs["mix_norm_g"]).reshape(1, D),
        "w_in": w_in_p,
        "attn_sinks": f(inputs["attn_sinks"]).reshape(1, 8),
        "w_spatial": f(inputs["w_spatial"])[0],
        "b_spatial": f(inputs["b_spatial"])[0],
        "gmlp_ln_g": f(inputs["gmlp_ln_g"]).reshape(1, 512),
        "gmlp_ln_b": f(inputs["gmlp_ln_b"]).reshape(1, 512),
        "attn_out_g": f(inputs["attn_out_g"]).reshape(1, 512),
        "gmlp_out_g": f(inputs["gmlp_out_g"]).reshape(1, 512),
        "w_out": f(inputs["w_out"])[0],
        "ffn_norm_g": f(inputs["ffn_norm_g"]).reshape(1, D),
        "w_group_router": f(inputs["w_group_router"])[0],
        "b_group_router": f(inputs["b_group_router"]).reshape(1, 4),
        "w_expert_router": f(inputs["w_expert_router"])[0],
        "b_expert_router": f(inputs["b_expert_router"]).reshape(1, 32),
        "w_gate": f(inputs["w_gate"])[0],
        "w_up": f(inputs["w_up"])[0],
        "w_down": f(inputs["w_down"])[0],
        "final_norm_g": f(inputs["final_norm_g"]).reshape(1, D),
    }
    xf = x.reshape(B * Sq, D)
    NBLK_H = TOK // 128
    in_maps = []
    for c in range(NCORES):
        t0 = c * TOK
        first = (t0 % Sq) == 0
        masks = np.zeros((128, 4, 512), np.float32)
        masks[:, 0, :] = np.tile(mcur, (1, 4))
        masks[:, 1, :] = np.tile(mprev, (1, 4))
        masks[:, 2, :] = 0.0 if first else np.tile(mprev, (1, 4))
        masks[:, 3, 0:128] = tril
        xh = np.zeros((128, D), np.float32) if first else xf[t0 - 128:t0]
        m = dict(common)
        m["x"] = np.ascontiguousarray(xf[t0:t0 + TOK])
        m["xh"] = np.ascontiguousarray(xh)
        m["masks"] = masks
        m["tokid"] = (np.arange(NBLK_H)[None, :] * 128 + np.arange(128)[:, None]).astype(np.int32)
        m["dst"] = np.stack([2 * m["tokid"], 2 * m["tokid"] + 1], axis=-1).astype(np.int32)
        m["ecap"] = (np.arange(32, dtype=np.float32) * TOK).reshape(1, 32)
        in_maps.append(m)
    return in_maps, (B, Sq)


def kernel(**inputs):
    in_maps, (B, Sq) = _prep_inputs(inputs)
    if "nc" not in _NC_CACHE:
        _NC_CACHE["nc"] = build_nc()
    nc = _NC_CACHE["nc"]
    res = run_bass_kernel_spmd(nc, in_maps, core_ids=list(range(NCORES)))
    out = np.concatenate([np.asarray(r["out"]) for r in res.results], axis=0)
    return out.reshape(B, Sq, D).astype(np.float32)
```

```python
import contextlib
import numpy as np
import concourse.bass as bass
import concourse.mybir as mybir
from concourse.bass_utils import run_bass_kernel_spmd

F32 = mybir.dt.float32
BF16 = mybir.dt.bfloat16
I32 = mybir.dt.int32
AF = mybir.ActivationFunctionType
ALU = mybir.AluOpType
AX = mybir.AxisListType

NCORES = 8
D = 1024
TOK = 4096
NBLK = TOK // 128
NE = 32
DE = 256
INP = 1792
RS_EPS = 1e-6
LN_EPS = 1e-5
MAGIC = 1597463007


PSUM_KEYS = frozenset(["pA", "pB", "pC", "pD", "pE", "pF", "pG", "pH"])


class Op:
    __slots__ = ("eng", "fn", "deps", "dma", "seq", "sig", "sem", "count", "region")

    def __init__(self, eng, fn, dma):
        self.eng = eng
        self.fn = fn
        self.dma = dma
        self.deps = []
        self.seq = 0
        self.sig = False
        self.sem = None
        self.count = 0


class Region:
    def __init__(self, thresh, parent, regs=None, invert=False):
        self.thresh = thresh
        self.regs = regs
        self.invert = invert
        self.items = []
        self.parent = parent
        self.end = {}


class Sched:
    def __init__(self, nc, es):
        self.nc = nc
        self.es = es
        self.engs = {"sp": nc.sync, "act": nc.scalar, "dve": nc.vector, "pe": nc.tensor, "pool": nc.gpsimd}
        self.esem = {e: es.enter_context(nc.semaphore("sem_" + e)) for e in ("act", "dve", "pe", "pool")}
        self.ops = []
        self.root = Region(None, None)
        self.cur = self.root
        self.regs = None
        self.suffix = ""
        self.lw = {}
        self.rd = {}
        self.dsem = {}

    def add(self, eng, fn, r=(), w=(), dma=None):
        if self.suffix:
            r = [k if self.is_global(k) else k + self.suffix for k in r]
            w = [k if self.is_global(k) else k + self.suffix for k in w]
        op = Op(eng, fn, dma)
        pr = [k for k in r if k in PSUM_KEYS]
        if pr:
            r = [k for k in r if k not in PSUM_KEYS]
            w = list(w) + pr
        deps = []
        for k in r:
            x = self.lw.get(k)
            if x is not None:
                deps.append(x)
        for k in w:
            x = self.lw.get(k)
            if x is not None:
                deps.append(x)
            deps.extend(self.rd.get(k, ()))
        op.deps = deps
        for k in r:
            self.rd.setdefault(k, []).append(op)
        for k in w:
            self.lw[k] = op
            self.rd[k] = []
        op.region = self.cur
        self.ops.append(op)
        self.cur.items.append(op)
        return op

    GLOBAL_KEYS = frozenset(["win", "wout", "identb", "identf", "g1b", "gmb0", "gmb1", "g2b", "gfb", "lngb", "lnbb", "bsb",
                             "esink", "wsT", "msk", "wr0", "wr1", "wr2", "wr3", "wr4", "rb0", "rb1", "trilf"])
    GLOBAL_PREFIXES = ("kT", "vaug", "xt", "h1t", "lg_", "h1d_", "hn2d_")

    def is_global(self, k):
        return k in PSUM_KEYS or k in self.GLOBAL_KEYS or k.startswith(self.GLOBAL_PREFIXES)

    def begin_if(self, thresh, regs=None, invert=False):
        r = Region(thresh, self.cur, regs if regs is not None else self.regs, invert)
        self.cur.items.append(r)
        self.cur = r

    def end_if(self):
        self.cur = self.cur.parent

    def emit(self):
        nc = self.nc
        import os
        mx = int(os.environ.get("K_MAXOPS", "0"))
        if mx:
            self.ops = self.ops[:mx]
        if os.environ.get("K_LISTOPS"):
            for i, op in enumerate(self.ops):
                print(i, op.eng, op.dma, getattr(op, "tag", ""))
        for op in self.ops:
            for d in op.deps:
                if d.dma is None:
                    d.sig = True
        cnt = {e: 0 for e in self.esem}
        dcnt = {}
        for op in self.ops:
            if op.dma is not None:
                if op.dma not in self.dsem:
                    self.dsem[op.dma] = self.es.enter_context(nc.semaphore("dsem_" + op.dma))
                    dcnt[op.dma] = 0
                dcnt[op.dma] += 16
                op.sem = self.dsem[op.dma]
                op.count = dcnt[op.dma]
            elif op.sig:
                cnt[op.eng] += 1
                op.seq = cnt[op.eng]
        def chain(reg):
            out = []
            while reg is not None and reg.parent is not None:
                out.append(reg)
                reg = reg.parent
            return out

        for op in self.ops:
            if op.dma is not None:
                key, val = ("d", op.dma), op.count
            elif op.sig:
                key, val = ("e", op.eng), op.seq
            else:
                continue
            for rg in chain(op.region):
                if rg.end.get(key, 0) < val:
                    rg.end[key] = val

        def dep_wait(d, waiter_region):
            if d.dma is not None:
                key, val, sem = ("d", d.dma), d.count, d.sem
            else:
                key, val, sem = ("e", d.eng), d.seq, self.esem[d.eng]
            mine = set(id(r) for r in chain(waiter_region))
            for rg in reversed(chain(d.region)):
                if id(rg) not in mine:
                    val = rg.end[key]
                    break
            return key, val, sem

        waited = {e: {} for e in self.engs}
        first_cnt = {}

        def emit_op(op):
            E = self.engs[op.eng]
            need = {}
            for d in op.deps:
                if d.dma is None and d.eng == "pe" and op.eng == "pe" and op.dma is None:
                    continue
                key, val, sem = dep_wait(d, op.region)
                if key not in need or need[key][1] < val:
                    need[key] = (sem, val)
            for key, (sem, val) in need.items():
                if waited[op.eng].get(key, 0) >= val:
                    continue
                waited[op.eng][key] = val
                E.wait_ge(sem, val)
            ins = op.fn()
            if op.dma is not None:
                ins.then_inc(op.sem, 16)
            elif op.sig:
                ins.then_inc(self.esem[op.eng], 1)

        def flat(items):
            for it in items:
                if isinstance(it, Region):
                    yield from flat(it.items)
                else:
                    yield it

        def compensate(region):
            esig = {}
            dsig = {}
            inside = set(id(o) for o in flat(region.items))
            needs = {}
            for op in flat(region.items):
                for d in op.deps:
                    if id(d) in inside:
                        continue
                    key, val, sem = dep_wait(d, region.parent)
                    nd = needs.setdefault(op.eng, {})
                    if key not in nd or nd[key][1] < val:
                        nd[key] = (sem, val)
            for eng, nd in needs.items():
                for key, (sem, val) in nd.items():
                    if waited[eng].get(key, 0) >= val:
                        continue
                    self.engs[eng].wait_ge(sem, val)
            for op in flat(region.items):
                if op.dma is not None:
                    d = dsig.setdefault(op.dma, [op.eng, op.count - 16, 0, op.sem])
                    d[2] += 16
                elif op.sig:
                    e = esig.setdefault(op.eng, [op.seq - 1, 0])
                    e[1] += 1
            for eng, (before, n) in esig.items():
                E = self.engs[eng]
                if before > 0:
                    E.wait_ge(self.esem[eng], before)
                while n > 0:
                    c = min(n, 112)
                    n -= c
                    E.sem_inc(self.esem[eng], c)
            for k, (eng, before, n, sem) in dsig.items():
                E = self.engs[eng]
                if before > 0:
                    E.wait_ge(sem, before)
                if eng == "pool":
                    while n > 0:
                        c = min(n, 240)
                        n -= c
                        i = self.ndummy
                        self.ndummy += 1
                        E.dma_start(out=self.dummy_d[0:1, i:i + 1], in_=self.dummy[0:1, 0:1]).then_inc(sem, c)
                else:
                    while n > 0:
                        c = min(n, 112)
                        n -= c
                        E.sem_inc(sem, c)

        def emit_items(items):
            for it in items:
                if isinstance(it, Region):
                    snap = {e: dict(v) for e, v in waited.items()}
                    if it.invert:
                        with nc.If_lt(it.regs, it.thresh + 1):
                            emit_items(it.items)
                        with nc.Else():
                            compensate(it)
                    else:
                        with nc.If_lt(it.regs, it.thresh + 1):
                            compensate(it)
                        with nc.Else():
                            emit_items(it.items)
                    for e in waited:
                        waited[e] = snap[e]
                else:
                    emit_op(it)

        emit_items(self.root.items)
        sp = nc.sync
        for k, sem in self.dsem.items():
            sp.wait_ge(sem, dcnt[k])
        return cnt

    def tt(self, eng, out, in0, in1, op, r, w):
        E = self.engs[eng]
        return self.add(eng, lambda: E.tensor_tensor(out=out, in0=in0, in1=in1, op=op), r, w)

    def ts(self, eng, out, in0, s1, s2, op0, op1, r, w):
        E = self.engs[eng]
        if op1 is None:
            return self.add(eng, lambda: E.tensor_scalar(out=out, in0=in0, scalar1=s1, scalar2=None, op0=op0), r, w)
        return self.add(eng, lambda: E.tensor_scalar(out=out, in0=in0, scalar1=s1, scalar2=s2, op0=op0, op1=op1), r, w)

    def stt(self, eng, out, in0, scalar, in1, op0, op1, r, w):
        E = self.engs[eng]
        return self.add(eng, lambda: E.scalar_tensor_tensor(out=out, in0=in0, scalar=scalar, in1=in1, op0=op0, op1=op1), r, w)

    def copy(self, eng, out, in_, r, w):
        E = self.engs[eng]
        if eng == "act":
            return self.add(eng, lambda: E.copy(out=out, in_=in_), r, w)
        return self.add(eng, lambda: E.tensor_copy(out=out, in_=in_), r, w)

    def actv(self, out, in_, func, r, w, scale=1.0, accum=None):
        E = self.nc.scalar
        if accum is None:
            return self.add("act", lambda: E.activation(out=out, in_=in_, func=func, scale=scale), r, w)
        return self.add("act", lambda: E.activation(out=out, in_=in_, func=func, scale=scale, accum_out=accum), r, w)

    def red(self, eng, out, in_, op, r, w):
        E = self.engs[eng]
        return self.add(eng, lambda: E.tensor_reduce(out=out, in_=in_, axis=AX.X, op=op), r, w)

    def mm(self, lst, r, w):
        T = self.nc.tensor

        def f():
            ins = None
            for (out, lhsT, rhs, st, sp) in lst:
                ins = T.matmul(out, lhsT=lhsT, rhs=rhs, start=st, stop=sp)
            return ins
        return self.add("pe", f, r, w)

    def tr(self, lst, r, w):
        T = self.nc.tensor

        def f():
            ins = None
            for (out, in_, ident) in lst:
                ins = T.transpose(out=out, in_=in_, identity=ident)
            return ins
        return self.add("pe", f, r, w)

    def dma(self, q, out, in_, r, w, stream, ncok=False):
        E = self.engs[q]
        if ncok:
            return self.add(q, lambda: E.dma_start(out=out, in_=in_, allow_slow_non_contiguous=True), r, w, dma=stream)
        return self.add(q, lambda: E.dma_start(out=out, in_=in_), r, w, dma=stream)


def build_nc(TOK=TOK, NE_RUN=NE):
    NBLK = TOK // 128
    BPP = NBLK
    CAP = TOK
    JMAX = CAP // 128
    nc = bass.Bass("TRN2", target_bir_lowering=False)

    def din(name, shape, dt=F32):
        return nc.dram_tensor(name, list(shape), dt, kind="ExternalInput").ap()

    x_d = din("x", [TOK, D])
    xh_d = din("xh", [128, D])
    masks_d = din("masks", [128, 4, 512])
    g1_d = din("mix_norm_g", [1, D])
    win_d = din("w_in", [D, INP])
    sinks_d = din("attn_sinks", [1, 8])
    wsp_d = din("w_spatial", [8, 128, 128])
    bsp_d = din("b_spatial", [8, 128])
    lng_d = din("gmlp_ln_g", [1, 512])
    lnb_d = din("gmlp_ln_b", [1, 512])
    ga_d = din("attn_out_g", [1, 512])
    gg_d = din("gmlp_out_g", [1, 512])
    wout_d = din("w_out", [D, D])
    g2_d = din("ffn_norm_g", [1, D])
    wgr_d = din("w_group_router", [D, 4])
    bgr_d = din("b_group_router", [1, 4])
    wer_d = din("w_expert_router", [4, D, 8])
    ber_d = din("b_expert_router", [1, 32])
    wg_d = din("w_gate", [NE, D, DE])
    wu_d = din("w_up", [NE, D, DE])
    wd_d = din("w_down", [NE, DE, D])
    gf_d = din("final_norm_g", [1, D])
    tokid_d = din("tokid", [128, NBLK], I32)
    ecap_d = din("ecap", [1, 32])
    dst_d = din("dst", [128, NBLK, 2], I32)
    out_d = nc.dram_tensor("out", [TOK, D], F32, kind="ExternalOutput").ap()
    h1_d = nc.dram_tensor("h1_scr", [TOK, D], F32, kind="Internal").ap()
    hn2_d = nc.dram_tensor("hn2_scr", [TOK, D], BF16, kind="Internal").ap()
    info_d = nc.dram_tensor("info_scr", [NE * CAP, 4], I32, kind="Internal").ap()
    wgb_d = nc.dram_tensor("wg_bf", [NE, D, DE], BF16, kind="Internal").ap()
    wub_d = nc.dram_tensor("wu_bf", [NE, D, DE], BF16, kind="Internal").ap()
    wdb_d = nc.dram_tensor("wd_bf", [NE, DE, D], BF16, kind="Internal").ap()
    y2_d = nc.dram_tensor("y2_scr", [2 * TOK, D], F32, kind="Internal").ap()

    es = contextlib.ExitStack()
    with es:
        def sb(name, shape, dt):
            return es.enter_context(nc.sbuf_tensor(name, list(shape), dt))

        def psum(name, shape, dt):
            return es.enter_context(nc.psum_tensor(name, list(shape), dt))

        S = Sched(nc, es)
        S.dummy = sb("dmy_sb", [128, 2], F32)
        S.dummy_d = nc.dram_tensor("dmy_scr", [1, 8192], F32, kind="Internal").ap()
        S.ndummy = 0

        win = sb("win", [128, 8, INP], BF16)
        wout = sb("wout", [128, 8, D], BF16)
        wgt = [sb(f"wgt{i}", [128, 8, DE], BF16) for i in range(2)]
        wup = [sb(f"wup{i}", [128, 8, DE], BF16) for i in range(2)]
        wdn = [sb(f"wdn{i}", [128, 2, D], BF16) for i in range(2)]
        g1b = sb("g1b", [128, D], F32)
        gmb = sb("gmb", [128, D], F32)
        g2b = sb("g2b", [128, D], F32)
        gfb = sb("gfb", [128, D], F32)
        lngb = sb("lngb", [128, 512], F32)
        lnbb = sb("lnbb", [128, 512], F32)
        bsb = sb("bsb", [128, 8], F32)
        esink = sb("esink", [128, 8], F32)
        wsT = sb("wsT", [128, 8, 128], BF16)
        msk = sb("msk", [128, 4, 512], BF16)
        trilf = sb("trilf", [128, 128], F32)
        identb = sb("identb", [128, 128], BF16)
        identf = sb("identf", [128, 128], F32)
        wr = sb("wr", [128, 8, 36], F32)
        rbias = sb("rbias", [128, 36], F32)

        h1t = [sb(f"h1t{i}", [128, D], F32) for i in range(2)]
        gates = sb("gates", [128, BPP, 32], F32)
        lg = sb("lg", [128, BPP, 36], F32)
        Mb = sb("Mb", [128, BPP, 32], BF16)
        Macc = sb("Macc", [128, BPP + 1, 32], BF16)
        slf = sb("slf", [128, BPP, 32], F32)
        tokid = sb("tokid_sb", [128, BPP], I32)
        ecap = sb("ecap_sb", [128, 32], F32)
        infoA = sb("infoA", [128, BPP, 4], F32)
        infoB = sb("infoB", [128, BPP, 4], F32)
        dstt = sb("dst_sb", [128, BPP, 2], I32)
        sAi = sb("sAi", [128, BPP], I32)
        sBi = sb("sBi", [128, BPP], I32)
        cnt_i = sb("cnt_i", [128, 32], I32)
        onesb = sb("onesb", [128, 128], BF16)
        utri = sb("utri", [128, 128], BF16)
        si = [sb(f"si{i}", [128, 4], I32) for i in range(8)]
        kT = sb("kT", [128, 3, 128], BF16)
        vaug = sb("vaug", [128, 3, 2, 65], BF16)

        xt = [sb(f"xt{i}", [128, D], F32) for i in range(2)]
        JUNK = [sb(f"junk{i}", [128, D], BF16) for i in range(2)]
        HN = [sb(f"hn{i}", [128, D], BF16) for i in range(2)]
        HNT = [sb(f"hnT{i}", [128, 8, 128], BF16) for i in range(2)]
        QT = [sb(f"qT{i}", [128, 4, 128], BF16) for i in range(2)]
        UT = [sb(f"u_t{i}", [128, 512], F32) for i in range(2)]
        VGT = [sb(f"vg_t{i}", [128, 512], F32) for i in range(2)]
        VNT = [sb(f"vn_t{i}", [128, 512], BF16) for i in range(2)]
        PT = [[[sb(f"p{i}{g}{kb}", [128, 4, 128], BF16) for kb in range(2)] for g in range(2)] for i in range(2)]
        AT = [sb(f"a_t{i}", [128, 8, 64], F32) for i in range(2)]
        GMT = [sb(f"gm_t{i}", [128, 8, 64], F32) for i in range(2)]
        MIXED = [sb(f"mixed{i}", [128, D], BF16) for i in range(2)]
        MIXEDT = [sb(f"mixedT{i}", [128, 8, 128], BF16) for i in range(2)]
        HN2 = [sb(f"hn2_{i}", [128, D], F32) for i in range(2)]
        HN2T32 = [sb(f"hn2T32_{i}", [128, 8, 128], F32) for i in range(2)]
        junk = JUNK[0]
        wsf = HN2[0][:].rearrange("p (h c) -> p h c", h=8)
        wsm = MIXED[0][:].rearrange("p (h c) -> p h c", h=8)
        hid = [sb(f"hid{i}", [128, 2, 128], BF16) for i in range(3)]
        sil = [sb(f"sil{i}", [128, 256], F32) for i in range(3)]
        woutf = wout[:].rearrange("p k n -> p (k n)")
        xg = [woutf[:, i * 1024:(i + 1) * 1024] for i in range(2)]
        xgT = [woutf[:, 2048 + i * 1024:2048 + (i + 1) * 1024].rearrange("p (k t) -> p k t", k=8) for i in range(2)]
        yt = [woutf[:, 4096 + i * 2048:4096 + (i + 1) * 2048].bitcast(F32) for i in range(2)]
        winf = win[:].rearrange("p k n -> p (k n)").bitcast(F32)
        ya = [winf[:, i * 1024:(i + 1) * 1024] for i in range(2)]
        yt = yt + [winf[:, i * 1024:(i + 1) * 1024] for i in range(2)]
        yt = yt + [winf[:, 2048:3072]]
        xg = xg + [winf[:, 3072:3584].bitcast(BF16)]
        xgT = xgT + [winf[:, 3584:4096].bitcast(BF16).rearrange("p (k t) -> p k t", k=8)]
        rt = winf[:, 2048:2048 + BPP * 96].rearrange("p (b c) -> p b c", c=96)
        yc = [winf[:, i * 2048:(i + 1) * 2048].rearrange("p (k d) -> p k d", k=2) for i in range(3)]
        woutf32 = woutf.bitcast(F32)
        hc = [woutf32[:, i * 1024:(i + 1) * 1024] for i in range(3)]
        STT = [sb(f"st{i}", [128, 64], F32) for i in range(2)]
        st = STT[0]

        pA32 = psum("pA", [128, 512], F32)
        pA = pA32[:].bitcast(BF16).rearrange("p (k t) -> p k t", k=8)
        pB = psum("pB", [128, 512], F32)
        pC = psum("pC", [128, 512], F32)
        pD = psum("pD", [128, 512], F32)
        pE = psum("pE", [128, 512], F32)
        pF = psum("pF", [128, 512], F32)
        pG = psum("pG", [128, 512], F32)
        pH = psum("pH", [128, 512], F32)
        pGb = pG[:].bitcast(BF16).rearrange("p (k t) -> p k t", k=8)
        pHb = pH[:].bitcast(BF16).rearrange("p (k t) -> p k t", k=8)

        S.dma("pool", win[:], win_d.rearrange("(k p) n -> p k n", p=128), [], ["win"], "win")
        S.dma("pool", wout[:], wout_d.rearrange("(k p) n -> p k n", p=128), [], ["wout"], "wout")
        S.dma("pool", msk[:], masks_d[:, :, :], [], ["msk"], "msk")
        S.dma("sp", trilf[:], masks_d[:, 3, 0:128], [], ["trilf"], "c0")
        S.dma("sp", g1b[:], g1_d[0:1, :].partition_broadcast(128), [], ["g1b"], "c1")
        S.dma("sp", gmb[:, 0:512], ga_d[0:1, :].partition_broadcast(128), [], ["gmb0"], "c2")
        S.dma("sp", gmb[:, 512:1024], gg_d[0:1, :].partition_broadcast(128), [], ["gmb1"], "c3")
        S.dma("sp", g2b[:], g2_d[0:1, :].partition_broadcast(128), [], ["g2b"], "c4")
        S.dma("sp", gfb[:], gf_d[0:1, :].partition_broadcast(128), [], ["gfb"], "c5")
        S.dma("sp", lngb[:], lng_d[0:1, :].partition_broadcast(128), [], ["lngb"], "c6")
        S.dma("sp", lnbb[:], lnb_d[0:1, :].partition_broadcast(128), [], ["lnbb"], "c7")
        S.dma("sp", bsb[:], bsp_d.rearrange("h t -> t h"), [], ["bsb"], "c8", ncok=True)
        S.dma("sp", esink[:], sinks_d[0:1, :].partition_broadcast(128), [], ["esink"], "c9")
        S.dma("sp", wsf, wsp_d.rearrange("h t c -> t h c"), [], ["hn2@0"], "c10")
        S.dma("sp", wr[:, :, 0:4], wgr_d.rearrange("(p k) n -> p k n", k=8), [], ["wr0"], "c11")
        for g in range(4):
            S.dma("sp", wr[:, :, 4 + 8 * g:12 + 8 * g], wer_d[g].rearrange("(p k) n -> p k n", k=8), [], [f"wr{g + 1}"], f"c{12 + g}")
        S.dma("sp", rbias[:, 0:4], bgr_d[0:1, :].partition_broadcast(128), [], ["rb0"], "c16")
        S.dma("sp", rbias[:, 4:36], ber_d[0:1, :].partition_broadcast(128), [], ["rb1"], "c17")
        WR = ["wr0", "wr1", "wr2", "wr3", "wr4"]
        S.dma("sp", tokid[:], tokid_d[:, :], [], ["tokid"], "c18")
        S.dma("sp", dstt[:], dst_d[:, :, :], [], ["dstt"], "c20")
        S.dma("sp", ecap[:], ecap_d[0:1, :].partition_broadcast(128), [], ["ecap"], "c19")
        S.add("pool", lambda: nc.gpsimd.memset(onesb[:], 1.0), [], ["onesb"])
        S.add("pool", lambda: nc.gpsimd.memset(utri[:], 1.0), [], ["utri"])
        S.add("pool", lambda: nc.gpsimd.affine_select(out=utri[:], in_=utri[:], pattern=[[1, 128]], compare_op=ALU.is_gt,
                                                      fill=0.0, base=0, channel_multiplier=-1), ["utri"], ["utri"])

        S.add("pool", lambda: nc.gpsimd.memset(S.dummy[:], 1.0), [], ["dummy"])
        S.add("pool", lambda: nc.gpsimd.memset(identf[:], 0.0), [], ["identf"])
        S.add("pool", lambda: nc.gpsimd.affine_select(out=identf[:], in_=identf[:], pattern=[[-1, 128]], compare_op=ALU.not_equal,
                                                      fill=1.0, base=0, channel_multiplier=1), ["identf"], ["identf"])
        S.copy("dve", identb[:], identf[:], ["identf"], ["identb"])
        S.add("pool", lambda: nc.gpsimd.memset(vaug[:], 1.0), [], ["vaug0", "vaug1", "vaug2"])
        S.actv(esink[:], esink[:], AF.Exp, ["esink"], ["esink"])
        S.tt("dve", wsm, wsf, trilf[:].unsqueeze(1).to_broadcast([128, 8, 128]), ALU.mult, ["hn2@0", "trilf"], ["mixed0@0", "mixed1@0"])
        S.tr([(pA[:, h, :], wsm[:, h, :], identb[:]) for h in range(8)], ["mixed0@0", "mixed1@0", "identb"], ["pA"])
        S.copy("dve", wsT[:], pA, ["pA"], ["wsT"])

        def rsqrt_chain(y, a, t, key, n_iter=2):
            S.actv(t, a, AF.Ln, [key + "a"], [key + "t"])
            S.actv(y, t, AF.Exp, [key + "t"], [key + "y"], scale=-0.5)

        def block(gb, halo=False):
            b = gb % BPP
            par = gb % 2
            S.suffix = "@%d" % par
            junk, hn, hnT, qT, u_t, vg_t, vn_t = JUNK[par], HN[par], HNT[par], QT[par], UT[par], VGT[par], VNT[par]
            pt, a_t, gm_t, mixed, mixedT, hn2, hn2T32, st = PT[par], AT[par], GMT[par], MIXED[par], MIXEDT[par], HN2[par], HN2T32[par], STT[par]
            slot = gb % 3
            pslot = (gb - 1) % 3
            xs = xt[gb % 2]
            xk = f"xt{gb % 2}"
            if halo:
                S.dma("sp", xs[:], xh_d[:, :], [], [xk], xk)
            else:
                S.dma("sp", xs[:], x_d[gb * 128:(gb + 1) * 128, :], [], [xk], xk)
            S.actv(junk[:], xs[:], AF.Square, [xk], ["junk", "st0"], scale=1.0 / 32, accum=st[:, 0:1])
            S.ts("dve", st[:, 1:2], st[:, 0:1], RS_EPS, None, ALU.add, None, ["st0"], ["r1a"])
            rsqrt_chain(st[:, 2:3], st[:, 1:2], st[:, 3:4], "r1")
            S.stt("dve", hn[:], xs[:], st[:, 2:3], g1b[:], ALU.mult, ALU.mult, [xk, "r1y", "g1b"], ["hn"])
            yield
            S.suffix = "@%d" % par
            S.tr([(pA[:, k, :], hn[:, k * 128:(k + 1) * 128], identb[:]) for k in range(8)], ["hn", "identb"], ["pA"])
            S.copy("act", hnT[:], pA, ["pA"], ["hnT"])
            yield
            S.suffix = "@%d" % par
            lst = []
            if not halo:
                for c in range(4):
                    for k in range(8):
                        lst.append((pB[:, c * 128:(c + 1) * 128], win[:, k, c * 128:(c + 1) * 128], hnT[:, k, :], k == 0, k == 7))
            for k in range(8):
                lst.append((pC[:, 0:128], win[:, k, 512:640], hnT[:, k, :], k == 0, k == 7))
            S.mm(lst, ["win", "hnT"], ["pB", "pC"])
            lst = []
            for k in range(8):
                lst.append((pF[:, 0:128], hnT[:, k, :], win[:, k, 640:768], k == 0, k == 7))
            if not halo:
                for k in range(8):
                    lst.append((pD[:, :], hnT[:, k, :], win[:, k, 768:1280], k == 0, k == 7))
                for k in range(8):
                    lst.append((pE[:, :], hnT[:, k, :], win[:, k, 1280:1792], k == 0, k == 7))
            S.mm(lst, ["win", "hnT"], ["pD", "pE", "pF"])
            S.copy("act", kT[:, slot, :], pC[:, 0:128], ["pC"], [f"kT{slot}"])
            S.copy("dve", vaug[:, slot, :, 0:64], pF[:, 0:128].rearrange("p (g d) -> p g d", g=2), ["pF"], [f"vaug{slot}"])
            if halo:
                return
            S.copy("dve", qT[:], pB[:].rearrange("p (c t) -> p c t", c=4), ["pB"], ["qT"])
            S.actv(u_t[:], pD[:], AF.Gelu, ["pD"], ["u_t"])
            S.actv(vg_t[:], pE[:], AF.Gelu, ["pE"], ["vg_t", "st8"], accum=st[:, 8:9])
            S.actv(junk[:, 0:512], vg_t[:], AF.Square, ["vg_t"], ["junk", "st9"], accum=st[:, 9:10])
            yield
            S.suffix = "@%d" % par
            for g in range(2):
                for kb in range(2):
                    ks = pslot if kb == 0 else slot
                    pp = pG if kb == 0 else pH
                    pk = "pG" if kb == 0 else "pH"
                    S.mm([(pp[:, :], kT[g * 64:(g + 1) * 64, ks, :], qT[g * 64:(g + 1) * 64, :, :], True, True)],
                         [f"kT{ks}", "qT"], [pk])
                    S.actv(pt[g][kb][:], pp[:].rearrange("p (c t) -> p c t", c=4), AF.Exp, [pk], [f"p{g}{kb}"], scale=0.125)
                    mi = (2 if gb == 0 else 1) if kb == 0 else 0
                    S.tt("pool", pt[g][kb][:], pt[g][kb][:], msk[:, mi, :].rearrange("p (c t) -> p c t", c=4), ALU.mult,
                         [f"p{g}{kb}", "msk"], [f"p{g}{kb}"])
            yield
            S.suffix = "@%d" % par
            for g in range(2):
                pv = pB if g == 0 else pC
                lst = []
                for c in range(4):
                    for kb in range(2):
                        ks = pslot if kb == 0 else slot
                        lst.append((pv[:, c * 65:(c + 1) * 65], pt[g][kb][:, c, :], vaug[:, ks, g, :], kb == 0, kb == 1))
                S.mm(lst, [f"p{g}0", f"p{g}1", f"vaug{pslot}", f"vaug{slot}"], ["pB" if g == 0 else "pC"])
            for g in range(2):
                pv = pB if g == 0 else pC
                pvk = "pB" if g == 0 else "pC"
                pv3 = pv[:, 0:260].rearrange("p (c e) -> p c e", c=4)
                S.tt("dve", st[:, 16 + 4 * g:20 + 4 * g], pv3[:, :, 64], esink[:, 4 * g:4 * g + 4], ALU.add, [pvk, "esink"], [f"den{g}"])
                S.add("dve", (lambda g=g: nc.vector.reciprocal(out=st[:, 24 + 4 * g:28 + 4 * g], in_=st[:, 16 + 4 * g:20 + 4 * g])),
                      [f"den{g}"], [f"rden{g}"])
                S.tt("dve", a_t[:, 4 * g:4 * g + 4, :], pv3[:, :, 0:64],
                     st[:, 24 + 4 * g:28 + 4 * g].unsqueeze(2).to_broadcast([128, 4, 64]), ALU.mult, [pvk, f"rden{g}"], [f"a_t{g}"])
            S.actv(junk[:, 0:512], a_t[:].rearrange("p h d -> p (h d)"), AF.Square, ["a_t0", "a_t1"], ["junk", "st10"], accum=st[:, 10:11])
            yield
            S.suffix = "@%d" % par
            S.ts("dve", st[:, 32:33], st[:, 8:9], 1.0 / 512, None, ALU.mult, None, ["st8"], ["lnm"])
            S.tt("dve", st[:, 33:34], st[:, 32:33], st[:, 32:33], ALU.mult, ["lnm"], ["lnm2"])
            S.stt("dve", st[:, 34:35], st[:, 9:10], 1.0 / 512, st[:, 33:34], ALU.mult, ALU.subtract, ["st9", "lnm2"], ["lnv"])
            S.ts("dve", st[:, 35:36], st[:, 34:35], LN_EPS, None, ALU.add, None, ["lnv"], ["r2a"])
            rsqrt_chain(st[:, 36:37], st[:, 35:36], st[:, 37:38], "r2")
            S.ts("dve", vg_t[:], vg_t[:], st[:, 32:33], st[:, 36:37], ALU.subtract, ALU.mult, ["vg_t", "lnm", "r2y"], ["vg_t"])
            S.tt("dve", vg_t[:], vg_t[:], lngb[:], ALU.mult, ["vg_t", "lngb"], ["vg_t"])
            S.tt("dve", vn_t[:], vg_t[:], lnbb[:], ALU.add, ["vg_t", "lnbb"], ["vn_t"])
            S.mm([(pD[:, h * 64:(h + 1) * 64], wsT[:, h, :], vn_t[:, h * 64:(h + 1) * 64], True, True) for h in range(8)],
                 ["wsT", "vn_t"], ["pD"])
            S.tt("dve", gm_t[:], pD[:].rearrange("p (h d) -> p h d", h=8), bsb[:].unsqueeze(2).to_broadcast([128, 8, 64]), ALU.add,
                 ["pD", "bsb"], ["gm_t"])
            gmf = gm_t[:].rearrange("p h d -> p (h d)")
            S.tt("dve", gmf, gmf, u_t[:], ALU.mult, ["gm_t", "u_t"], ["gm_t"])
            S.actv(junk[:, 0:512], gmf, AF.Square, ["gm_t"], ["junk", "st11"], accum=st[:, 11:12])
            yield
            S.suffix = "@%d" % par
            S.ts("dve", st[:, 40:42], st[:, 10:12], 1.0 / 512, RS_EPS, ALU.mult, ALU.add, ["st10", "st11"], ["r3a"])
            rsqrt_chain(st[:, 42:44], st[:, 40:42], st[:, 44:46], "r3")
            S.stt("dve", mixed[:, 0:512], a_t[:].rearrange("p h d -> p (h d)"), st[:, 42:43], gmb[:, 0:512], ALU.mult, ALU.mult,
                  ["a_t0", "a_t1", "r3y", "gmb0"], ["mixed0"])
            S.stt("dve", mixed[:, 512:1024], gmf, st[:, 43:44], gmb[:, 512:1024], ALU.mult, ALU.mult,
                  ["gm_t", "r3y", "gmb1"], ["mixed1"])
            S.tr([(pGb[:, k, :], mixed[:, k * 128:(k + 1) * 128], identb[:]) for k in range(8)], ["mixed0", "mixed1", "identb"], ["pG"])
            S.copy("act", mixedT[:], pGb, ["pG"], ["mixedT"])
            yield
            S.suffix = "@%d" % par
            lst = []
            for n, pp in enumerate((pG, pH)):
                for k in range(8):
                    lst.append((pp[:, :], mixedT[:, k, :], wout[:, k, n * 512:(n + 1) * 512], k == 0, k == 7))
            S.mm(lst, ["mixedT", "wout"], ["pG", "pH"])
            h1s = h1t[gb % 2]
            h1k = f"h1t{gb % 2}"
            S.tt("dve", h1s[:, 0:512], pG[:], xs[:, 0:512], ALU.add, ["pG", xk], [h1k + "a"])
            S.tt("dve", h1s[:, 512:1024], pH[:], xs[:, 512:1024], ALU.add, ["pH", xk], [h1k + "b"])
            S.dma("sp", h1_d[gb * 128:(gb + 1) * 128, :], h1s[:], [h1k + "a", h1k + "b"], [f"h1d_{gb}"], f"h1st{gb % 2}")
            S.actv(junk[:], h1s[:], AF.Square, [h1k + "a", h1k + "b"], ["junk", "st12"], scale=1.0 / 32, accum=st[:, 12:13])
            S.ts("dve", st[:, 48:49], st[:, 12:13], RS_EPS, None, ALU.add, None, ["st12"], ["r4a"])
            rsqrt_chain(st[:, 49:50], st[:, 48:49], st[:, 50:51], "r4")
            S.stt("dve", hn2[:], h1s[:], st[:, 49:50], g2b[:], ALU.mult, ALU.mult, [h1k + "a", h1k + "b", "r4y", "g2b"], ["hn2"])
            S.dma("pool", hn2_d[gb * 128:(gb + 1) * 128, :], hn2[:], ["hn2"], [f"hn2d_{gb}"], f"hn2st{par}")
            yield
            S.suffix = "@%d" % par
            pDE = [pD, pD, pD, pD, pE, pE, pE, pE]
            S.tr([(pDE[k][:, (k % 4) * 128:(k % 4 + 1) * 128], hn2[:, bass.ds(k, 128, step=8)], identf[:]) for k in range(8)],
                 ["hn2", "identf"], ["pD", "pE"])
            S.copy("act", hn2T32[:, 0:4, :], pD[:].rearrange("p (k t) -> p k t", k=4), ["pD"], ["hn2T32a"])
            S.copy("act", hn2T32[:, 4:8, :], pE[:].rearrange("p (k t) -> p k t", k=4), ["pE"], ["hn2T32b"])
            S.mm([(pF[:, 0:36], hn2T32[:, k, :], wr[:, k, :], k == 0, k == 7) for k in range(8)], ["hn2T32a", "hn2T32b"] + WR, ["pF"])
            S.tt("dve", lg[:, b, :], pF[:, 0:36], rbias[:], ALU.add, ["pF", "rb0", "rb1"], [f"lg_{b}"])

        def routing():
            LG = [f"lg_{b}" for b in range(BPP)]
            gl = lg[:, :, 0:4]
            el = lg[:, :, 4:36].rearrange("p b (j e) -> p b j e", j=4)
            gmax = rt[:, :, 0:1]
            ohg = rt[:, :, 1:5]
            gex = rt[:, :, 5:9]
            gsum = rt[:, :, 9:10]
            gw = rt[:, :, 10:11]
            tmp = rt[:, :, 16:48].rearrange("p b (j e) -> p b j e", j=4)
            esel = rt[:, :, 48:56]
            m1 = rt[:, :, 11:12]
            m2 = rt[:, :, 12:13]
            eq1 = rt[:, :, 56:64]
            e2 = rt[:, :, 64:72]
            eq2 = rt[:, :, 72:80]
            dd = rt[:, :, 13:14]
            w1 = rt[:, :, 14:15]
            w2 = rt[:, :, 15:16]
            wn = rt[:, :, 80:88]
            S.red("dve", rt[:, :, 0], gl, ALU.max, LG, ["gmax"])
            S.tt("dve", ohg, gl, gmax.to_broadcast([128, BPP, 4]), ALU.is_equal, LG + ["gmax"], ["ohg"])
            S.tt("dve", gex, gl, gmax.to_broadcast([128, BPP, 4]), ALU.subtract, LG + ["gmax"], ["gex"])
            S.actv(gex, gex, AF.Exp, ["gex"], ["gex"])
            S.red("dve", rt[:, :, 9], gex, ALU.add, ["gex"], ["gsum"])
            S.add("dve", lambda: nc.vector.reciprocal(out=gw, in_=gsum), ["gsum"], ["gw"])
            S.tt("dve", tmp, el, ohg.unsqueeze(3).to_broadcast([128, BPP, 4, 8]), ALU.mult, LG + ["ohg"], ["tmp"])
            S.red("dve", esel, tmp.rearrange("p b j e -> p b e j"), ALU.add, ["tmp"], ["esel"])
            S.red("dve", rt[:, :, 11], esel, ALU.max, ["esel"], ["m1"])
            S.tt("dve", eq1, esel, m1.to_broadcast([128, BPP, 8]), ALU.is_equal, ["esel", "m1"], ["eq1"])
            S.stt("dve", e2, eq1, -1e30, esel, ALU.mult, ALU.add, ["eq1", "esel"], ["e2"])
            S.red("dve", rt[:, :, 12], e2, ALU.max, ["e2"], ["m2"])
            S.tt("dve", eq2, e2, m2.to_broadcast([128, BPP, 8]), ALU.is_equal, ["e2", "m2"], ["eq2"])
            S.tt("dve", dd, m2, m1, ALU.subtract, ["m1", "m2"], ["dd"])
            S.actv(dd, dd, AF.Exp, ["dd"], ["dd"])
            S.ts("dve", w1, dd, 1.0, None, ALU.add, None, ["dd"], ["w1"])
            S.add("dve", lambda: nc.vector.reciprocal(out=w1, in_=w1), ["w1"], ["w1"])
            S.tt("dve", w2, dd, w1, ALU.mult, ["dd", "w1"], ["w2"])
            S.tt("dve", w1, w1, gw, ALU.mult, ["w1", "gw"], ["w1"])
            S.tt("dve", w2, w2, gw, ALU.mult, ["w2", "gw"], ["w2"])
            S.tt("dve", eq1, eq1, w1.to_broadcast([128, BPP, 8]), ALU.mult, ["eq1", "w1"], ["eq1"])
            S.tt("dve", eq2, eq2, w2.to_broadcast([128, BPP, 8]), ALU.mult, ["eq2", "w2"], ["eq2"])
            S.tt("dve", wn, eq1, eq2, ALU.add, ["eq1", "eq2"], ["wn"])
            for j in range(4):
                S.tt("dve", gates[:, :, j * 8:(j + 1) * 8], wn, ohg[:, :, j:j + 1].to_broadcast([128, BPP, 8]), ALU.mult,
                     ["wn", "ohg"], ["gates"])

        def compaction():
            Mf = rt[:, :, 16:48]
            S.add("dve", lambda: nc.vector.tensor_single_scalar(out=Mf, in_=gates[:], scalar=0.0, op=ALU.is_gt), ["gates", "tmp"], ["tmp"])
            S.copy("dve", Mb[:], Mf, ["tmp"], ["Mb"])
            S.add("pool", lambda: nc.gpsimd.memset(Macc[:, 0, :], 0.0), [], ["Macc"])
            for b in range(BPP):
                S.tt("dve", Macc[:, b + 1, :], Macc[:, b, :], Mb[:, b, :], ALU.add, ["Macc", "Mb"], ["Macc"])
            for b in range(BPP):
                bank, bk = (pB, "pB") if (b // 16) % 2 == 0 else (pC, "pC")
                o = bank[:, (b % 16) * 32:(b % 16 + 1) * 32]
                S.mm([(o, utri[:], Mb[:, b, :], True, False), (o, onesb[:], Macc[:, b, :], False, True)], ["utri", "onesb", "Mb", "Macc"], [bk])
                if b % 16 == 15 or b == BPP - 1:
                    b0 = (b // 16) * 16
                    nb = b - b0 + 1
                    S.tt("dve", slf[:, b0:b0 + nb, :], bank[:, 0:nb * 32].rearrange("p (b e) -> p b e", e=32),
                         ecap[:].unsqueeze(1).to_broadcast([128, nb, 32]), ALU.add, [bk, "ecap"], [f"slf{b0}"])
            SLF = [f"slf{b0}" for b0 in range(0, BPP, 16)]
            S.mm([(pD[:, 0:32], onesb[:], Macc[:, BPP, :], True, True)], ["onesb", "Macc"], ["pD"])
            S.copy("dve", cnt_i[:], pD[:, 0:32], ["pD"], ["cnt_i"])
            sel = rt[:, :, 48:80]
            S.stt("dve", sel, slf[:], 1.0, Mf, ALU.add, ALU.mult, SLF + ["tmp"], ["sel"])
            S.ts("dve", sel, sel, -1.0, None, ALU.add, None, ["sel"], ["sel"])
            sA = rt[:, :, 0]
            sB = rt[:, :, 1]
            eqm = rt[:, :, 16:48]
            S.red("dve", sA, sel, ALU.max, ["sel"], ["sA"])
            S.tt("dve", eqm, sel, rt[:, :, 0:1].to_broadcast([128, BPP, 32]), ALU.is_equal, ["sel", "sA", "tmp"], ["tmp"])
            S.tt("dve", slf[:], eqm, gates[:], ALU.mult, ["tmp", "gates"] + SLF, SLF)
            S.red("dve", infoA[:, :, 2], slf[:], ALU.add, SLF, ["infoA1"])
            S.stt("dve", sel, eqm, -1e9, sel, ALU.mult, ALU.add, ["tmp", "sel"], ["sel"])
            S.red("dve", sB, sel, ALU.max, ["sel"], ["sB"])
            S.tt("dve", eqm, sel, rt[:, :, 1:2].to_broadcast([128, BPP, 32]), ALU.is_equal, ["sel", "sB", "tmp"], ["tmp"])
            S.tt("dve", slf[:], eqm, gates[:], ALU.mult, ["tmp", "gates"] + SLF, SLF)
            S.red("dve", infoB[:, :, 2], slf[:], ALU.add, SLF, ["infoB1"])
            S.copy("dve", infoA[:].bitcast(I32)[:, :, 0], tokid[:], ["tokid"], ["infoA0"])
            S.copy("dve", infoB[:].bitcast(I32)[:, :, 0], tokid[:], ["tokid"], ["infoB0"])
            S.copy("dve", infoA[:].bitcast(I32)[:, :, 1], dstt[:, :, 0], ["dstt"], ["infoA0"])
            S.copy("dve", infoB[:].bitcast(I32)[:, :, 1], dstt[:, :, 1], ["dstt"], ["infoB0"])
            S.add("pool", lambda: nc.gpsimd.memset(infoA[:, :, 3], 0.0), [], ["infoA0"])
            S.add("pool", lambda: nc.gpsimd.memset(infoB[:, :, 3], 0.0), [], ["infoB0"])
            S.copy("dve", sAi[:], sA, ["sA"], ["sAi"])
            S.copy("dve", sBi[:], sB, ["sB"], ["sBi"])
            zt = junk[:].bitcast(F32)
            S.add("pool", lambda: nc.gpsimd.memset(zt, 0.0), ["junk@0"], ["junk@0"])
            S.add("pool", lambda: nc.gpsimd.memset(zt.bitcast(I32).rearrange("p (r c) -> p r c", c=4)[:, :, 0], TOK), ["junk@0"], ["junk@0"])
            S.add("pool", lambda: nc.gpsimd.memset(zt.bitcast(I32).rearrange("p (r c) -> p r c", c=4)[:, :, 1], 2 * TOK), ["junk@0"], ["junk@0"])
            iv = info_d.rearrange("(p r) c -> p (r c)", p=128)
            ncol = NE * CAP * 4 // 128
            zw = min(512, ncol)
            for c0 in range(0, ncol, zw):
                S.dma("sp", iv[:, c0:c0 + zw], zt.bitcast(I32)[:, 0:zw], ["junk@0"], ["info"], f"iz{c0 // zw % 8}")
            for b in range(BPP):
                S.add("pool", (lambda b=b: nc.gpsimd.indirect_dma_start(
                    out=info_d[:, :], out_offset=bass.IndirectOffsetOnAxis(ap=sAi[:, b:b + 1], axis=0), in_=infoA[:].bitcast(I32)[:, b, :], in_offset=None)),
                    ["sAi", "infoA0", "infoA1", "info"], [f"infoA_{b}"], dma="scat")
                S.add("pool", (lambda b=b: nc.gpsimd.indirect_dma_start(
                    out=info_d[:, :], out_offset=bass.IndirectOffsetOnAxis(ap=sBi[:, b:b + 1], axis=0), in_=infoB[:].bitcast(I32)[:, b, :], in_offset=None)),
                    ["sBi", "infoB0", "infoB1", "info"], [f"infoB_{b}"], dma="scat")

        H1ALL = [f"h1d_{gb}" for gb in range(NBLK)]
        INFO_ALL = [f"infoA_{b}" for b in range(BPP)] + [f"infoB_{b}" for b in range(BPP)]
        HN2D = [f"hn2d_{gb}" for gb in range(NBLK)]

        def convert_expert(e):
            S.dma("pool", wgb_d[e], wg_d[e], [], [f"wcv_{e}g"], f"wcv{e % 4}")
            S.dma("pool", wub_d[e], wu_d[e], [], [f"wcv_{e}u"], f"wcv{e % 4}")
            S.dma("pool", wdb_d[e], wd_d[e], [], [f"wcv_{e}d"], f"wcv{e % 4}")

        def load_expert(e):
            s = e % 2
            WCV = [f"wcv_{ee}{t}" for ee in range(e % 4, NE, 4) for t in "gud"]
            S.dma("sp", wgt[s][:], wgb_d[e].rearrange("(p k) n -> p k n", k=8), WCV, [f"wgt{s}"], f"wgt{s}")
            S.dma("sp", wup[s][:], wub_d[e].rearrange("(p k) n -> p k n", k=8), WCV, [f"wup{s}"], f"wup{s}")
            S.dma("sp", wdn[s][:], wdb_d[e].rearrange("(m p) n -> p m n", p=128), WCV, [f"wdn{s}"], f"wdn{s}")

        tile_it = [0]

        bc_reg = nc.gpsimd.alloc_register("bc")
        nc.gpsimd.reg_mov(bc_reg, TOK - 1)
        bc2_reg = nc.gpsimd.alloc_register("bc2")
        nc.gpsimd.reg_mov(bc2_reg, 2 * TOK - 1)

        tile_q = {}
        J1_INLINE = min(3, JMAX)

        def tile_A(e, j):
            for _ in tile_A_gen(e, j):
                pass

        def tile_A_gen(e, j):
            s = e % 2
            q = j % 2
            if j < J1_INLINE:
                qx = j
                qb = j if j < 2 else 4
            else:
                qx = q
                qb = 2 + q
            qs = qb if (j >= J1_INLINE or e % 2 == 0) else (5 + j)
            tile_q[(e, j)] = (qb, qs)
            r0 = e * CAP + j * 128
            S.dma("sp", si[qs][:], info_d[r0:r0 + 128, :], INFO_ALL, [f"si{qs}"], f"si{qs}")
            S.add("pool", (lambda qx=qx, qb=qb: nc.gpsimd.indirect_dma_start(
                out=xg[qx], out_offset=None, in_=hn2_d[:, :], in_offset=bass.IndirectOffsetOnAxis(ap=si[qs][:, 0:1], axis=0),
                bounds_check=bc_reg, oob_is_err=False)),
                [f"si{qs}"] + HN2D, [f"xg{qx}"], dma=f"xg{qx}")
            yield
            ptr, ptrk = (pA, "pA") if q == 0 else (pHb, "pH")
            S.tr([(ptr[:, k, :], xg[qx][:, bass.ds(k, 128, step=8)], identb[:]) for k in range(8)], [f"xg{qx}", "identb"], [ptrk])
            yield
            S.copy("act", xgT[qx], ptr, [ptrk], [f"xgT{qx}"])
            yield
            pgu, pguk = (pB, "pB") if q == 0 else (pC, "pC")
            lst = []
            for m in range(2):
                for k in range(8):
                    lst.append((pgu[:, m * 128:(m + 1) * 128], wgt[s][:, k, m * 128:(m + 1) * 128], xgT[qx][:, k, :], k == 0, k == 7))
            for m in range(2):
                for k in range(8):
                    lst.append((pgu[:, 256 + m * 128:256 + (m + 1) * 128], wup[s][:, k, m * 128:(m + 1) * 128], xgT[qx][:, k, :], k == 0, k == 7))
            S.mm(lst, [f"wgt{s}", f"wup{s}", f"xgT{qx}"], [pguk])
            yield
            S.actv(sil[qx][:], pgu[:, 0:256], AF.Silu, [pguk], [f"sil{qx}"])
            S.tt("dve", hid[qx][:].rearrange("p m t -> p (m t)"), pgu[:, 256:512], sil[qx][:], ALU.mult, [pguk, f"sil{qx}"], [f"hid{qx}"])
            yield
            pd = (pD, pE) if q == 0 else (pF, pG)
            pdk = ("pD", "pE") if q == 0 else ("pF", "pG")
            lst = []
            for n in range(2):
                for m in range(2):
                    lst.append((pd[n][:, :], hid[qx][:, m, :], wdn[s][:, m, n * 512:(n + 1) * 512], m == 0, m == 1))
            S.mm(lst, [f"hid{qx}", f"wdn{s}"], list(pdk))
            yield
            S.actv(yt[qb][:, 0:512], pd[0][:], AF.Copy, [pdk[0], f"si{qs}"], [f"yt{qb}a"], scale=si[qs][:, 2:3].bitcast(F32))
            S.ts("dve", yt[qb][:, 512:1024], pd[1][:], si[qs][:, 2:3].bitcast(F32), None, ALU.mult, None, [pdk[1], f"si{qs}"], [f"yt{qb}b"])

        b_cnt = {}

        def tile_B(e, j):
            q, qs = tile_q[(e, j)]
            c = b_cnt.get((e, j), 0)
            b_cnt[(e, j)] = c + 1
            S.add("pool", (lambda q=q, qs=qs: nc.gpsimd.indirect_dma_start(
                out=y2_d[:, :], out_offset=bass.IndirectOffsetOnAxis(ap=si[qs][:, 1:2], axis=0), in_=yt[q], in_offset=None,
                bounds_check=bc2_reg, oob_is_err=False)),
                [f"yt{q}a", f"yt{q}b", f"si{qs}"], [f"y2_{e}_{j}_{c}"], dma=f"yst{q}")
            Y2ALL.append(f"y2_{e}_{j}_{c}")

        Y2ALL = []
        EMAP = {mybir.EngineType.Activation: "act", mybir.EngineType.DVE: "dve", mybir.EngineType.PE: "pe",
                mybir.EngineType.Pool: "pool", mybir.EngineType.SP: "sp"}

        def moe_sparse():
            regs = nc.alloc_registers("n_e")
            RS = [regs, regs]
            NUNC = min(3, JMAX)
            J1 = J1_INLINE

            def cond(e, j, fn):
                if j < NUNC:
                    fn(e, j)
                else:
                    S.begin_if(j * 128, RS[e % 2])
                    fn(e, j)
                    S.end_if()

            def tail(e, j=None):
                if j is None:
                    j = J1
                if j >= JMAX:
                    return
                S.begin_if(j * 128, regs)
                tile_A(e, j)
                if j > J1:
                    tile_B(e, j - 1)
                if j + 1 < JMAX:
                    tail(e, j + 1)
                    S.begin_if((j + 1) * 128, regs, invert=True)
                    tile_B(e, j)
                    S.end_if()
                else:
                    tile_B(e, j)
                S.end_if()

            assert NUNC == J1
            def run_gens(gens):
                hold = [0, 0, 2][:len(gens)]
                while gens:
                    alive = []
                    for gi, g in enumerate(gens):
                        if hold[gi] > 0:
                            hold[gi] -= 1
                            alive.append((g, hold[gi]))
                            continue
                        try:
                            next(g)
                            alive.append((g, 0))
                        except StopIteration:
                            pass
                    gens = [g for g, _ in alive]
                    hold = [h for _, h in alive]

            def front(e):
                gens = [tile_A_gen(e, jj) for jj in range(NUNC)]
                for g in gens:
                    next(g)
                return gens

            gens = front(0)
            for e in range(NE_RUN):
                for r in regs:
                    S.add(EMAP[r.engine], (lambda r=r, e=e: nc.reg_load(r, cnt_i[0:1, e:e + 1])), ["cnt_i"], [])
                if e + 1 < NE_RUN:
                    load_expert(e + 1)
                run_gens(gens)
                tail(e)
                if e + 1 < NE_RUN:
                    gens = front(e + 1)
                for jj in range(J1):
                    tile_B(e, jj)

        NCL = 3

        def combine_block(b):
            q = b % NCL
            S.dma("sp", hc[q], h1_d[b * 128:(b + 1) * 128, :], H1ALL + Y2ALL, [f"hc{q}"], f"hc{q}")
            S.dma("sp", yc[q], y2_d[2 * b * 128:2 * (b + 1) * 128, :].rearrange("(p k) d -> p k d", k=2), Y2ALL, [f"yc{q}"], f"yc{q}")
            yield
            S.tt("pool", yc[q][:, 0, :], yc[q][:, 0, :], yc[q][:, 1, :], ALU.add, [f"yc{q}"], [f"yc{q}"])
            yield
            S.tt("dve", hc[q], hc[q], yc[q][:, 0, :], ALU.add, [f"hc{q}", f"yc{q}"], [f"hc{q}"])
            yield
            k0 = 40 + 4 * q
            S.actv(junk[:], hc[q], AF.Square, [f"hc{q}"], ["junk@0", f"f{q}s"], scale=1.0 / 32, accum=st[:, k0:k0 + 1])
            yield
            S.ts("dve", st[:, k0 + 1:k0 + 2], st[:, k0:k0 + 1], RS_EPS, None, ALU.add, None, [f"f{q}s"], [f"f{q}a"])
            rsqrt_chain(st[:, k0 + 2:k0 + 3], st[:, k0 + 1:k0 + 2], st[:, k0 + 3:k0 + 4], f"f{q}")
            S.stt("dve", hc[q], hc[q], st[:, k0 + 2:k0 + 3], gfb[:], ALU.mult, ALU.mult, [f"hc{q}", f"f{q}y", "gfb"], [f"hc{q}"])
            yield
            S.dma("sp", out_d[b * 128:(b + 1) * 128, :], hc[q], [f"hc{q}"], [], f"ost{q}")

        def combine():
            def clane(start):
                for b in range(start, NBLK, NCL):
                    yield from combine_block(b)
            lanes = [clane(i) for i in range(NCL)]
            for i in range(NCL):
                for _ in range(2 * (NCL - 1 - i)):
                    next(lanes[i])
            while lanes:
                for ln in list(lanes):
                    try:
                        next(ln)
                    except StopIteration:
                        lanes.remove(ln)

        for _ in block(-1, halo=True):
            pass

        def lane(start):
            for gb in range(start, NBLK, 2):
                if gb < NE:
                    sv = S.suffix
                    S.suffix = ""
                    convert_expert(gb)
                    S.suffix = sv
                yield from block(gb)

        lanes = [lane(0), lane(1)]
        for _ in range(4):
            next(lanes[0])
        while lanes:
            for ln in list(lanes):
                try:
                    next(ln)
                except StopIteration:
                    lanes.remove(ln)
        S.suffix = ""
        for e in range(NBLK, NE):
            convert_expert(e)
        routing()
        compaction()
        S.add("pool", lambda: nc.gpsimd.memset(st[:, 60:61], 0.0), ["win", "wout"],
              ["win", "wout", "xg0", "xg1", "xgT0", "xgT1", "yt0a", "yt0b", "yt1a", "yt1b", "yt2a", "yt2b", "yt3a", "yt3b", "yt4a", "yt4b", "xg2", "xgT2", "yc0", "yc1", "yc2", "hc0", "hc1", "hc2"])
        S.add("pool", lambda: nc.gpsimd.memset(woutf[:, 0:2048], 0.0), ["xg0", "xg1"], ["xg0", "xg1"])
        S.add("pool", lambda: nc.gpsimd.memset(xg[2], 0.0), ["xg2"], ["xg2"])
        load_expert(0)
        moe_sparse()
        combine()
        S.emit()
        globals()['_LAST_NDUMMY'] = S.ndummy
    return nc


_NC_CACHE = {}


def _prep_inputs(inputs):
    f = lambda a: np.ascontiguousarray(np.asarray(a, dtype=np.float32))
    x = f(inputs["x"])
    B, Sq, _ = x.shape
    w_in = f(inputs["w_in"])[0]
    perm = []
    for c in range(4):
        perm += list(range(c * 64, c * 64 + 64)) + list(range((4 + c) * 64, (4 + c) * 64 + 64))
    perm += list(range(512, INP))
    w_in_p = np.ascontiguousarray(w_in[:, perm])
    s_idx = np.arange(128)[:, None]
    q_idx = np.arange(128)[None, :]
    mcur = (s_idx <= q_idx).astype(np.float32)
    mprev = (s_idx > q_idx).astype(np.float32)
    tril = (q_idx <= s_idx).astype(np.float32)
    common = {
        "mix_norm_g": f(inputs["mix_norm_g"]).reshape(1, D),
        "w_in": w_in_p,
        "attn_sinks": f(inputs["attn_sinks"]).reshape(1, 8),
        "w_spatial": f(inputs["w_spatial"])[0],
        "b_spatial": f(inputs["b_spatial"])[0],
        "gmlp_ln_g": f(inputs["gmlp_ln_g"]).reshape(1, 512),
        "gmlp_ln_b": f(inputs["gmlp_ln_b"]).reshape(1, 512),
        "attn_out_g": f(inputs["attn_out_g"]).reshape(1, 512),
        "gmlp_out_g": f(inputs["gmlp_out_g"]).reshape(1, 512),
        "w_out": f(inputs["w_out"])[0],
        "ffn_norm_g": f(inputs["ffn_norm_g"]).reshape(1, D),
        "w_group_router": f(inputs["w_group_router"])[0],
        "b_group_router": f(inputs["b_group_router"]).reshape(1, 4),
        "w_expert_router": f(inputs["w_expert_router"])[0],
        "b_expert_router": f(inputs["b_expert_router"]).reshape(1, 32),
        "w_gate": f(inputs["w_gate"])[0],
        "w_up": f(inputs["w_up"])[0],
        "w_down": f(inputs["w_down"])[0],
        "final_norm_g": f(inputs["final_norm_g"]).reshape(1, D),
    }
    xf = x.reshape(B * Sq, D)
    NBLK_H = TOK // 128
    in_maps = []
    for c in range(NCORES):
        t0 = c * TOK
        first = (t0 % Sq) == 0
        masks = np.zeros((128, 4, 512), np.float32)
        masks[:, 0, :] = np.tile(mcur, (1, 4))
        masks[:, 1, :] = np.tile(mprev, (1, 4))
        masks[:, 2, :] = 0.0 if first else np.tile(mprev, (1, 4))
        masks[:, 3, 0:128] = tril
        xh = np.zeros((128, D), np.float32) if first else xf[t0 - 128:t0]
        m = dict(common)
        m["x"] = np.ascontiguousarray(xf[t0:t0 + TOK])
        m["xh"] = np.ascontiguousarray(xh)
        m["masks"] = masks
        m["tokid"] = (np.arange(NBLK_H)[None, :] * 128 + np.arange(128)[:, None]).astype(np.int32)
        m["dst"] = np.stack([2 * m["tokid"], 2 * m["tokid"] + 1], axis=-1).astype(np.int32)
        m["ecap"] = (np.arange(32, dtype=np.float32) * TOK).reshape(1, 32)
        in_maps.append(m)
    return in_maps, (B, Sq)


def kernel(**inputs):
    in_maps, (B, Sq) = _prep_inputs(inputs)
    if "nc" not in _NC_CACHE:
        _NC_CACHE["nc"] = build_nc()
    nc = _NC_CACHE["nc"]
    res = run_bass_kernel_spmd(nc, in_maps, core_ids=list(range(NCORES)))
    out = np.concatenate([np.asarray(r["out"]) for r in res.results], axis=0)
    return out.reshape(B, Sq, D).astype(np.float32)
```

```python
import contextlib
import numpy as np
import concourse.bass as bass
import concourse.mybir as mybir
from concourse.bass_utils import run_bass_kernel_spmd

F32 = mybir.dt.float32
BF16 = mybir.dt.bfloat16
I32 = mybir.dt.int32
AF = mybir.ActivationFunctionType
ALU = mybir.AluOpType
AX = mybir.AxisListType

NCORES = 8
D = 1024
TOK = 4096
NBLK = TOK // 128
NE = 32
DE = 256
INP = 1792
RS_EPS = 1e-6
LN_EPS = 1e-5
MAGIC = 1597463007


PSUM_KEYS = frozenset(["pA", "pB", "pC", "pD", "pE", "pF", "pG", "pH"])


class Op:
    __slots__ = ("eng", "fn", "deps", "dma", "seq", "sig", "sem", "count", "region")

    def __init__(self, eng, fn, dma):
        self.eng = eng
        self.fn = fn
        self.dma = dma
        self.deps = []
        self.seq = 0
        self.sig = False
        self.sem = None
        self.count = 0


class Region:
    def __init__(self, thresh, parent, regs=None, invert=False):
        self.thresh = thresh
        self.regs = regs
        self.invert = invert
        self.items = []
        self.parent = parent
        self.end = {}


class Sched:
    def __init__(self, nc, es):
        self.nc = nc
        self.es = es
        self.engs = {"sp": nc.sync, "act": nc.scalar, "dve": nc.vector, "pe": nc.tensor, "pool": nc.gpsimd}
        self.esem = {e: es.enter_context(nc.semaphore("sem_" + e)) for e in ("act", "dve", "pe", "pool")}
        self.ops = []
        self.root = Region(None, None)
        self.cur = self.root
        self.regs = None
        self.suffix = ""
        self.lw = {}
        self.rd = {}
        self.dsem = {}

    def add(self, eng, fn, r=(), w=(), dma=None):
        if self.suffix:
            r = [k if self.is_global(k) else k + self.suffix for k in r]
            w = [k if self.is_global(k) else k + self.suffix for k in w]
        op = Op(eng, fn, dma)
        pr = [k for k in r if k in PSUM_KEYS]
        if pr:
            r = [k for k in r if k not in PSUM_KEYS]
            w = list(w) + pr
        deps = []
        for k in r:
            x = self.lw.get(k)
            if x is not None:
                deps.append(x)
        for k in w:
            x = self.lw.get(k)
            if x is not None:
                deps.append(x)
            deps.extend(self.rd.get(k, ()))
        op.deps = deps
        for k in r:
            self.rd.setdefault(k, []).append(op)
        for k in w:
            self.lw[k] = op
            self.rd[k] = []
        op.region = self.cur
        self.ops.append(op)
        self.cur.items.append(op)
        return op

    GLOBAL_KEYS = frozenset(["win", "wout", "identb", "identf", "g1b", "gmb0", "gmb1", "g2b", "gfb", "lngb", "lnbb", "bsb",
                             "esink", "wsT", "msk", "wr0", "wr1", "wr2", "wr3", "wr4", "rb0", "rb1", "trilf"])
    GLOBAL_PREFIXES = ("kT", "vaug", "xt", "h1t", "lg_", "h1d_", "hn2d_")

    def is_global(self, k):
        return k in PSUM_KEYS or k in self.GLOBAL_KEYS or k.startswith(self.GLOBAL_PREFIXES)

    def begin_if(self, thresh, regs=None, invert=False):
        r = Region(thresh, self.cur, regs if regs is not None else self.regs, invert)
        self.cur.items.append(r)
        self.cur = r

    def end_if(self):
        self.cur = self.cur.parent

    def emit(self):
        nc = self.nc
        import os
        mx = int(os.environ.get("K_MAXOPS", "0"))
        if mx:
            self.ops = self.ops[:mx]
        if os.environ.get("K_LISTOPS"):
            for i, op in enumerate(self.ops):
                print(i, op.eng, op.dma, getattr(op, "tag", ""))
        for op in self.ops:
            for d in op.deps:
                if d.dma is None:
                    d.sig = True
        cnt = {e: 0 for e in self.esem}
        dcnt = {}
        for op in self.ops:
            if op.dma is not None:
                if op.dma not in self.dsem:
                    self.dsem[op.dma] = self.es.enter_context(nc.semaphore("dsem_" + op.dma))
                    dcnt[op.dma] = 0
                dcnt[op.dma] += 16
                op.sem = self.dsem[op.dma]
                op.count = dcnt[op.dma]
            elif op.sig:
                cnt[op.eng] += 1
                op.seq = cnt[op.eng]
        def chain(reg):
            out = []
            while reg is not None and reg.parent is not None:
                out.append(reg)
                reg = reg.parent
            return out

        for op in self.ops:
            if op.dma is not None:
                key, val = ("d", op.dma), op.count
            elif op.sig:
                key, val = ("e", op.eng), op.seq
            else:
                continue
            for rg in chain(op.region):
                if rg.end.get(key, 0) < val:
                    rg.end[key] = val

        def dep_wait(d, waiter_region):
            if d.dma is not None:
                key, val, sem = ("d", d.dma), d.count, d.sem
            else:
                key, val, sem = ("e", d.eng), d.seq, self.esem[d.eng]
            mine = set(id(r) for r in chain(waiter_region))
            for rg in reversed(chain(d.region)):
                if id(rg) not in mine:
                    val = rg.end[key]
                    break
            return key, val, sem

        waited = {e: {} for e in self.engs}
        first_cnt = {}

        def emit_op(op):
            E = self.engs[op.eng]
            need = {}
            for d in op.deps:
                if d.dma is None and d.eng == "pe" and op.eng == "pe" and op.dma is None:
                    continue
                key, val, sem = dep_wait(d, op.region)
                if key not in need or need[key][1] < val:
                    need[key] = (sem, val)
            for key, (sem, val) in need.items():
                if waited[op.eng].get(key, 0) >= val:
                    continue
                waited[op.eng][key] = val
                E.wait_ge(sem, val)
            ins = op.fn()
            if op.dma is not None:
                ins.then_inc(op.sem, 16)
            elif op.sig:
                ins.then_inc(self.esem[op.eng], 1)

        def flat(items):
            for it in items:
                if isinstance(it, Region):
                    yield from flat(it.items)
                else:
                    yield it

        def compensate(region):
            esig = {}
            dsig = {}
            inside = set(id(o) for o in flat(region.items))
            needs = {}
            for op in flat(region.items):
                for d in op.deps:
                    if id(d) in inside:
                        continue
                    key, val, sem = dep_wait(d, region.parent)
                    nd = needs.setdefault(op.eng, {})
                    if key not in nd or nd[key][1] < val:
                        nd[key] = (sem, val)
            for eng, nd in needs.items():
                for key, (sem, val) in nd.items():
                    if waited[eng].get(key, 0) >= val:
                        continue
                    self.engs[eng].wait_ge(sem, val)
            for op in flat(region.items):
                if op.dma is not None:
                    d = dsig.setdefault(op.dma, [op.eng, op.count - 16, 0, op.sem])
                    d[2] += 16
                elif op.sig:
                    e = esig.setdefault(op.eng, [op.seq - 1, 0])
                    e[1] += 1
            for eng, (before, n) in esig.items():
                E = self.engs[eng]
                if before > 0:
                    E.wait_ge(self.esem[eng], before)
                while n > 0:
                    c = min(n, 112)
                    n -= c
                    E.sem_inc(self.esem[eng], c)
            for k, (eng, before, n, sem) in dsig.items():
                E = self.engs[eng]
                if before > 0:
                    E.wait_ge(sem, before)
                if eng == "pool":
                    while n > 0:
                        c = min(n, 240)
                        n -= c
                        i = self.ndummy
                        self.ndummy += 1
                        E.dma_start(out=self.dummy_d[0:1, i:i + 1], in_=self.dummy[0:1, 0:1]).then_inc(sem, c)
                else:
                    while n > 0:
                        c = min(n, 112)
                        n -= c
                        E.sem_inc(sem, c)

        def emit_items(items):
            for it in items:
                if isinstance(it, Region):
                    snap = {e: dict(v) for e, v in waited.items()}
                    if it.invert:
                        with nc.If_lt(it.regs, it.thresh + 1):
                            emit_items(it.items)
                        with nc.Else():
                            compensate(it)
                    else:
                        with nc.If_lt(it.regs, it.thresh + 1):
                            compensate(it)
                        with nc.Else():
                            emit_items(it.items)
                    for e in waited:
                        waited[e] = snap[e]
                else:
                    emit_op(it)

        emit_items(self.root.items)
        sp = nc.sync
        for k, sem in self.dsem.items():
            sp.wait_ge(sem, dcnt[k])
        return cnt

    def tt(self, eng, out, in0, in1, op, r, w):
        E = self.engs[eng]
        return self.add(eng, lambda: E.tensor_tensor(out=out, in0=in0, in1=in1, op=op), r, w)

    def ts(self, eng, out, in0, s1, s2, op0, op1, r, w):
        E = self.engs[eng]
        if op1 is None:
            return self.add(eng, lambda: E.tensor_scalar(out=out, in0=in0, scalar1=s1, scalar2=None, op0=op0), r, w)
        return self.add(eng, lambda: E.tensor_scalar(out=out, in0=in0, scalar1=s1, scalar2=s2, op0=op0, op1=op1), r, w)

    def stt(self, eng, out, in0, scalar, in1, op0, op1, r, w):
        E = self.engs[eng]
        return self.add(eng, lambda: E.scalar_tensor_tensor(out=out, in0=in0, scalar=scalar, in1=in1, op0=op0, op1=op1), r, w)

    def copy(self, eng, out, in_, r, w):
        E = self.engs[eng]
        if eng == "act":
            return self.add(eng, lambda: E.copy(out=out, in_=in_), r, w)
        return self.add(eng, lambda: E.tensor_copy(out=out, in_=in_), r, w)

    def actv(self, out, in_, func, r, w, scale=1.0, accum=None):
        E = self.nc.scalar
        if accum is None:
            return self.add("act", lambda: E.activation(out=out, in_=in_, func=func, scale=scale), r, w)
        return self.add("act", lambda: E.activation(out=out, in_=in_, func=func, scale=scale, accum_out=accum), r, w)

    def red(self, eng, out, in_, op, r, w):
        E = self.engs[eng]
        return self.add(eng, lambda: E.tensor_reduce(out=out, in_=in_, axis=AX.X, op=op), r, w)

    def mm(self, lst, r, w):
        T = self.nc.tensor

        def f():
            ins = None
            for (out, lhsT, rhs, st, sp) in lst:
                ins = T.matmul(out, lhsT=lhsT, rhs=rhs, start=st, stop=sp)
            return ins
        return self.add("pe", f, r, w)

    def tr(self, lst, r, w):
        T = self.nc.tensor

        def f():
            ins = None
            for (out, in_, ident) in lst:
                ins = T.transpose(out=out, in_=in_, identity=ident)
            return ins
        return self.add("pe", f, r, w)

    def dma(self, q, out, in_, r, w, stream, ncok=False):
        E = self.engs[q]
        if ncok:
            return self.add(q, lambda: E.dma_start(out=out, in_=in_, allow_slow_non_contiguous=True), r, w, dma=stream)
        return self.add(q, lambda: E.dma_start(out=out, in_=in_), r, w, dma=stream)


def build_nc(TOK=TOK, NE_RUN=NE):
    NBLK = TOK // 128
    BPP = NBLK
    CAP = TOK
    JMAX = CAP // 128
    nc = bass.Bass("TRN2", target_bir_lowering=False)

    def din(name, shape, dt=F32):
        return nc.dram_tensor(name, list(shape), dt, kind="ExternalInput").ap()

    x_d = din("x", [TOK, D])
    xh_d = din("xh", [128, D])
    masks_d = din("masks", [128, 4, 512])
    g1_d = din("mix_norm_g", [1, D])
    win_d = din("w_in", [D, INP])
    sinks_d = din("attn_sinks", [1, 8])
    wsp_d = din("w_spatial", [8, 128, 128])
    bsp_d = din("b_spatial", [8, 128])
    lng_d = din("gmlp_ln_g", [1, 512])
    lnb_d = din("gmlp_ln_b", [1, 512])
    ga_d = din("attn_out_g", [1, 512])
    gg_d = din("gmlp_out_g", [1, 512])
    wout_d = din("w_out", [D, D])
    g2_d = din("ffn_norm_g", [1, D])
    wgr_d = din("w_group_router", [D, 4])
    bgr_d = din("b_group_router", [1, 4])
    wer_d = din("w_expert_router", [4, D, 8])
    ber_d = din("b_expert_router", [1, 32])
    wg_d = din("w_gate", [NE, D, DE])
    wu_d = din("w_up", [NE, D, DE])
    wd_d = din("w_down", [NE, DE, D])
    gf_d = din("final_norm_g", [1, D])
    tokid_d = din("tokid", [128, NBLK], I32)
    ecap_d = din("ecap", [1, 32])
    dst_d = din("dst", [128, NBLK, 2], I32)
    out_d = nc.dram_tensor("out", [TOK, D], F32, kind="ExternalOutput").ap()
    h1_d = nc.dram_tensor("h1_scr", [TOK, D], F32, kind="Internal").ap()
    hn2_d = nc.dram_tensor("hn2_scr", [TOK, D], BF16, kind="Internal").ap()
    info_d = nc.dram_tensor("info_scr", [NE * CAP, 4], I32, kind="Internal").ap()
    wgb_d = nc.dram_tensor("wg_bf", [NE, D, DE], BF16, kind="Internal").ap()
    wub_d = nc.dram_tensor("wu_bf", [NE, D, DE], BF16, kind="Internal").ap()
    wdb_d = nc.dram_tensor("wd_bf", [NE, DE, D], BF16, kind="Internal").ap()
    y2_d = nc.dram_tensor("y2_scr", [2 * TOK, D], F32, kind="Internal").ap()

    es = contextlib.ExitStack()
    with es:
        def sb(name, shape, dt):
            return es.enter_context(nc.sbuf_tensor(name, list(shape), dt))

        def psum(name, shape, dt):
            return es.enter_context(nc.psum_tensor(name, list(shape), dt))

        S = Sched(nc, es)
        S.dummy = sb("dmy_sb", [128, 2], F32)
        S.dummy_d = nc.dram_tensor("dmy_scr", [1, 8192], F32, kind="Internal").ap()
        S.ndummy = 0

        win = sb("win", [128, 8, INP], BF16)
        wout = sb("wout", [128, 8, D], BF16)
        wgt = [sb(f"wgt{i}", [128, 8, DE], BF16) for i in range(2)]
        wup = [sb(f"wup{i}", [128, 8, DE], BF16) for i in range(2)]
        wdn = [sb(f"wdn{i}", [128, 2, D], BF16) for i in range(2)]
        g1b = sb("g1b", [128, D], F32)
        gmb = sb("gmb", [128, D], F32)
        g2b = sb("g2b", [128, D], F32)
        gfb = sb("gfb", [128, D], F32)
        lngb = sb("lngb", [128, 512], F32)
        lnbb = sb("lnbb", [128, 512], F32)
        bsb = sb("bsb", [128, 8], F32)
        esink = sb("esink", [128, 8], F32)
        wsT = sb("wsT", [128, 8, 128], BF16)
        msk = sb("msk", [128, 4, 512], BF16)
        trilf = sb("trilf", [128, 128], F32)
        identb = sb("identb", [128, 128], BF16)
        identf = sb("identf", [128, 128], F32)
        wr = sb("wr", [128, 8, 36], F32)
        rbias = sb("rbias", [128, 36], F32)

        h1t = [sb(f"h1t{i}", [128, D], F32) for i in range(2)]
        gates = sb("gates", [128, BPP, 32], F32)
        lg = sb("lg", [128, BPP, 36], F32)
        Mb = sb("Mb", [128, BPP, 32], BF16)
        Macc = sb("Macc", [128, BPP + 1, 32], BF16)
        slf = sb("slf", [128, BPP, 32], F32)
        tokid = sb("tokid_sb", [128, BPP], I32)
        ecap = sb("ecap_sb", [128, 32], F32)
        infoA = sb("infoA", [128, BPP, 4], F32)
        infoB = sb("infoB", [128, BPP, 4], F32)
        dstt = sb("dst_sb", [128, BPP, 2], I32)
        sAi = sb("sAi", [128, BPP], I32)
        sBi = sb("sBi", [128, BPP], I32)
        cnt_i = sb("cnt_i", [128, 32], I32)
        onesb = sb("onesb", [128, 128], BF16)
        utri = sb("utri", [128, 128], BF16)
        si = [sb(f"si{i}", [128, 4], I32) for i in range(8)]
        kT = sb("kT", [128, 3, 128], BF16)
        vaug = sb("vaug", [128, 3, 2, 65], BF16)

        xt = [sb(f"xt{i}", [128, D], F32) for i in range(2)]
        JUNK = [sb(f"junk{i}", [128, D], BF16) for i in range(2)]
        HN = [sb(f"hn{i}", [128, D], BF16) for i in range(2)]
        HNT = [sb(f"hnT{i}", [128, 8, 128], BF16) for i in range(2)]
        QT = [sb(f"qT{i}", [128, 4, 128], BF16) for i in range(2)]
        UT = [sb(f"u_t{i}", [128, 512], F32) for i in range(2)]
        VGT = [sb(f"vg_t{i}", [128, 512], F32) for i in range(2)]
        VNT = [sb(f"vn_t{i}", [128, 512], BF16) for i in range(2)]
        PT = [[[sb(f"p{i}{g}{kb}", [128, 4, 128], BF16) for kb in range(2)] for g in range(2)] for i in range(2)]
        AT = [sb(f"a_t{i}", [128, 8, 64], F32) for i in range(2)]
        GMT = [sb(f"gm_t{i}", [128, 8, 64], F32) for i in range(2)]
        MIXED = [sb(f"mixed{i}", [128, D], BF16) for i in range(2)]
        MIXEDT = [sb(f"mixedT{i}", [128, 8, 128], BF16) for i in range(2)]
        HN2 = [sb(f"hn2_{i}", [128, D], F32) for i in range(2)]
        HN2T32 = [sb(f"hn2T32_{i}", [128, 8, 128], F32) for i in range(2)]
        junk = JUNK[0]
        wsf = HN2[0][:].rearrange("p (h c) -> p h c", h=8)
        wsm = MIXED[0][:].rearrange("p (h c) -> p h c", h=8)
        hid = [sb(f"hid{i}", [128, 2, 128], BF16) for i in range(3)]
        sil = [sb(f"sil{i}", [128, 256], F32) for i in range(3)]
        woutf = wout[:].rearrange("p k n -> p (k n)")
        xg = [woutf[:, i * 1024:(i + 1) * 1024] for i in range(2)]
        xgT = [woutf[:, 2048 + i * 1024:2048 + (i + 1) * 1024].rearrange("p (k t) -> p k t", k=8) for i in range(2)]
        yt = [woutf[:, 4096 + i * 2048:4096 + (i + 1) * 2048].bitcast(F32) for i in range(2)]
        winf = win[:].rearrange("p k n -> p (k n)").bitcast(F32)
        ya = [winf[:, i * 1024:(i + 1) * 1024] for i in range(2)]
        yt = yt + [winf[:, i * 1024:(i + 1) * 1024] for i in range(2)]
        yt = yt + [winf[:, 2048:3072]]
        xg = xg + [winf[:, 3072:3584].bitcast(BF16)]
        xgT = xgT + [winf[:, 3584:4096].bitcast(BF16).rearrange("p (k t) -> p k t", k=8)]
        rt = winf[:, 2048:2048 + BPP * 96].rearrange("p (b c) -> p b c", c=96)
        yc = [winf[:, i * 2048:(i + 1) * 2048].rearrange("p (k d) -> p k d", k=2) for i in range(3)]
        woutf32 = woutf.bitcast(F32)
        hc = [woutf32[:, i * 1024:(i + 1) * 1024] for i in range(3)]
        STT = [sb(f"st{i}", [128, 64], F32) for i in range(2)]
        st = STT[0]

        pA32 = psum("pA", [128, 512], F32)
        pA = pA32[:].bitcast(BF16).rearrange("p (k t) -> p k t", k=8)
        pB = psum("pB", [128, 512], F32)
        pC = psum("pC", [128, 512], F32)
        pD = psum("pD", [128, 512], F32)
        pE = psum("pE", [128, 512], F32)
        pF = psum("pF", [128, 512], F32)
        pG = psum("pG", [128, 512], F32)
        pH = psum("pH", [128, 512], F32)
        pHb = pH[:].bitcast(BF16).rearrange("p (k t) -> p k t", k=8)

        S.dma("pool", win[:], win_d.rearrange("(k p) n -> p k n", p=128), [], ["win"], "win")
        S.dma("pool", wout[:], wout_d.rearrange("(k p) n -> p k n", p=128), [], ["wout"], "wout")
        S.dma("pool", msk[:], masks_d[:, :, :], [], ["msk"], "msk")
        S.dma("sp", trilf[:], masks_d[:, 3, 0:128], [], ["trilf"], "c0")
        S.dma("sp", g1b[:], g1_d[0:1, :].partition_broadcast(128), [], ["g1b"], "c1")
        S.dma("sp", gmb[:, 0:512], ga_d[0:1, :].partition_broadcast(128), [], ["gmb0"], "c2")
        S.dma("sp", gmb[:, 512:1024], gg_d[0:1, :].partition_broadcast(128), [], ["gmb1"], "c3")
        S.dma("sp", g2b[:], g2_d[0:1, :].partition_broadcast(128), [], ["g2b"], "c4")
        S.dma("sp", gfb[:], gf_d[0:1, :].partition_broadcast(128), [], ["gfb"], "c5")
        S.dma("sp", lngb[:], lng_d[0:1, :].partition_broadcast(128), [], ["lngb"], "c6")
        S.dma("sp", lnbb[:], lnb_d[0:1, :].partition_broadcast(128), [], ["lnbb"], "c7")
        S.dma("sp", bsb[:], bsp_d.rearrange("h t -> t h"), [], ["bsb"], "c8", ncok=True)
        S.dma("sp", esink[:], sinks_d[0:1, :].partition_broadcast(128), [], ["esink"], "c9")
        S.dma("sp", wsf, wsp_d.rearrange("h t c -> t h c"), [], ["hn2@0"], "c10")
        S.dma("sp", wr[:, :, 0:4], wgr_d.rearrange("(p k) n -> p k n", k=8), [], ["wr0"], "c11")
        for g in range(4):
            S.dma("sp", wr[:, :, 4 + 8 * g:12 + 8 * g], wer_d[g].rearrange("(p k) n -> p k n", k=8), [], [f"wr{g + 1}"], f"c{12 + g}")
        S.dma("sp", rbias[:, 0:4], bgr_d[0:1, :].partition_broadcast(128), [], ["rb0"], "c16")
        S.dma("sp", rbias[:, 4:36], ber_d[0:1, :].partition_broadcast(128), [], ["rb1"], "c17")
        WR = ["wr0", "wr1", "wr2", "wr3", "wr4"]
        S.dma("sp", tokid[:], tokid_d[:, :], [], ["tokid"], "c18")
        S.dma("sp", dstt[:], dst_d[:, :, :], [], ["dstt"], "c20")
        S.dma("sp", ecap[:], ecap_d[0:1, :].partition_broadcast(128), [], ["ecap"], "c19")
        S.add("pool", lambda: nc.gpsimd.memset(onesb[:], 1.0), [], ["onesb"])
        S.add("pool", lambda: nc.gpsimd.memset(utri[:], 1.0), [], ["utri"])
        S.add("pool", lambda: nc.gpsimd.affine_select(out=utri[:], in_=utri[:], pattern=[[1, 128]], compare_op=ALU.is_gt,
                                                      fill=0.0, base=0, channel_multiplier=-1), ["utri"], ["utri"])

        S.add("pool", lambda: nc.gpsimd.memset(S.dummy[:], 1.0), [], ["dummy"])
        S.add("pool", lambda: nc.gpsimd.memset(identf[:], 0.0), [], ["identf"])
        S.add("pool", lambda: nc.gpsimd.affine_select(out=identf[:], in_=identf[:], pattern=[[-1, 128]], compare_op=ALU.not_equal,
                                                      fill=1.0, base=0, channel_multiplier=1), ["identf"], ["identf"])
        S.copy("dve", identb[:], identf[:], ["identf"], ["identb"])
        S.add("pool", lambda: nc.gpsimd.memset(vaug[:], 1.0), [], ["vaug0", "vaug1", "vaug2"])
        S.actv(esink[:], esink[:], AF.Exp, ["esink"], ["esink"])
        S.tt("dve", wsm, wsf, trilf[:].unsqueeze(1).to_broadcast([128, 8, 128]), ALU.mult, ["hn2@0", "trilf"], ["mixed0@0", "mixed1@0"])
        S.tr([(pA[:, h, :], wsm[:, h, :], identb[:]) for h in range(8)], ["mixed0@0", "mixed1@0", "identb"], ["pA"])
        S.copy("dve", wsT[:], pA, ["pA"], ["wsT"])

        def rsqrt_chain(y, a, t, key, n_iter=2):
            S.actv(t, a, AF.Ln, [key + "a"], [key + "t"])
            S.actv(y, t, AF.Exp, [key + "t"], [key + "y"], scale=-0.5)

        def block(gb, halo=False):
            b = gb % BPP
            par = gb % 2
            S.suffix = "@%d" % par
            junk, hn, hnT, qT, u_t, vg_t, vn_t = JUNK[par], HN[par], HNT[par], QT[par], UT[par], VGT[par], VNT[par]
            pt, a_t, gm_t, mixed, mixedT, hn2, hn2T32, st = PT[par], AT[par], GMT[par], MIXED[par], MIXEDT[par], HN2[par], HN2T32[par], STT[par]
            slot = gb % 3
            pslot = (gb - 1) % 3
            xs = xt[gb % 2]
            xk = f"xt{gb % 2}"
            if halo:
                S.dma("sp", xs[:], xh_d[:, :], [], [xk], xk)
            else:
                S.dma("sp", xs[:], x_d[gb * 128:(gb + 1) * 128, :], [], [xk], xk)
            S.actv(junk[:], xs[:], AF.Square, [xk], ["junk", "st0"], scale=1.0 / 32, accum=st[:, 0:1])
            S.ts("dve", st[:, 1:2], st[:, 0:1], RS_EPS, None, ALU.add, None, ["st0"], ["r1a"])
            rsqrt_chain(st[:, 2:3], st[:, 1:2], st[:, 3:4], "r1")
            S.stt("dve", hn[:], xs[:], st[:, 2:3], g1b[:], ALU.mult, ALU.mult, [xk, "r1y", "g1b"], ["hn"])
            yield
            S.suffix = "@%d" % par
            S.tr([(pA[:, k, :], hn[:, k * 128:(k + 1) * 128], identb[:]) for k in range(8)], ["hn", "identb"], ["pA"])
            S.copy("act", hnT[:], pA, ["pA"], ["hnT"])
            yield
            S.suffix = "@%d" % par
            lst = []
            if not halo:
                for c in range(4):
                    for k in range(8):
                        lst.append((pB[:, c * 128:(c + 1) * 128], win[:, k, c * 128:(c + 1) * 128], hnT[:, k, :], k == 0, k == 7))
            for k in range(8):
                lst.append((pC[:, 0:128], win[:, k, 512:640], hnT[:, k, :], k == 0, k == 7))
            S.mm(lst, ["win", "hnT"], ["pB", "pC"])
            lst = []
            for k in range(8):
                lst.append((pF[:, 0:128], hnT[:, k, :], win[:, k, 640:768], k == 0, k == 7))
            if not halo:
                for k in range(8):
                    lst.append((pD[:, :], hnT[:, k, :], win[:, k, 768:1280], k == 0, k == 7))
                for k in range(8):
                    lst.append((pE[:, :], hnT[:, k, :], win[:, k, 1280:1792], k == 0, k == 7))
            S.mm(lst, ["win", "hnT"], ["pD", "pE", "pF"])
            S.copy("act", kT[:, slot, :], pC[:, 0:128], ["pC"], [f"kT{slot}"])
            S.copy("dve", vaug[:, slot, :, 0:64], pF[:, 0:128].rearrange("p (g d) -> p g d", g=2), ["pF"], [f"vaug{slot}"])
            if halo:
                return
            S.copy("dve", qT[:], pB[:].rearrange("p (c t) -> p c t", c=4), ["pB"], ["qT"])
            S.actv(u_t[:], pD[:], AF.Gelu, ["pD"], ["u_t"])
            S.actv(vg_t[:], pE[:], AF.Gelu, ["pE"], ["vg_t", "st8"], accum=st[:, 8:9])
            S.actv(junk[:, 0:512], vg_t[:], AF.Square, ["vg_t"], ["junk", "st9"], accum=st[:, 9:10])
            yield
            S.suffix = "@%d" % par
            for g in range(2):
                for kb in range(2):
                    ks = pslot if kb == 0 else slot
                    pp = pG if kb == 0 else pH
                    pk = "pG" if kb == 0 else "pH"
                    S.mm([(pp[:, :], kT[g * 64:(g + 1) * 64, ks, :], qT[g * 64:(g + 1) * 64, :, :], True, True)],
                         [f"kT{ks}", "qT"], [pk])
                    S.actv(pt[g][kb][:], pp[:].rearrange("p (c t) -> p c t", c=4), AF.Exp, [pk], [f"p{g}{kb}"], scale=0.125)
                    mi = (2 if gb == 0 else 1) if kb == 0 else 0
                    S.tt("pool", pt[g][kb][:], pt[g][kb][:], msk[:, mi, :].rearrange("p (c t) -> p c t", c=4), ALU.mult,
                         [f"p{g}{kb}", "msk"], [f"p{g}{kb}"])
            yield
            S.suffix = "@%d" % par
            for g in range(2):
                pv = pB if g == 0 else pC
                lst = []
                for c in range(4):
                    for kb in range(2):
                        ks = pslot if kb == 0 else slot
                        lst.append((pv[:, c * 65:(c + 1) * 65], pt[g][kb][:, c, :], vaug[:, ks, g, :], kb == 0, kb == 1))
                S.mm(lst, [f"p{g}0", f"p{g}1", f"vaug{pslot}", f"vaug{slot}"], ["pB" if g == 0 else "pC"])
            for g in range(2):
                pv = pB if g == 0 else pC
                pvk = "pB" if g == 0 else "pC"
                pv3 = pv[:, 0:260].rearrange("p (c e) -> p c e", c=4)
                S.tt("dve", st[:, 16 + 4 * g:20 + 4 * g], pv3[:, :, 64], esink[:, 4 * g:4 * g + 4], ALU.add, [pvk, "esink"], [f"den{g}"])
                S.add("dve", (lambda g=g: nc.vector.reciprocal(out=st[:, 24 + 4 * g:28 + 4 * g], in_=st[:, 16 + 4 * g:20 + 4 * g])),
                      [f"den{g}"], [f"rden{g}"])
                S.tt("dve", a_t[:, 4 * g:4 * g + 4, :], pv3[:, :, 0:64],
                     st[:, 24 + 4 * g:28 + 4 * g].unsqueeze(2).to_broadcast([128, 4, 64]), ALU.mult, [pvk, f"rden{g}"], [f"a_t{g}"])
            S.actv(junk[:, 0:512], a_t[:].rearrange("p h d -> p (h d)"), AF.Square, ["a_t0", "a_t1"], ["junk", "st10"], accum=st[:, 10:11])
            yield
            S.suffix = "@%d" % par
            S.ts("dve", st[:, 32:33], st[:, 8:9], 1.0 / 512, None, ALU.mult, None, ["st8"], ["lnm"])
            S.tt("dve", st[:, 33:34], st[:, 32:33], st[:, 32:33], ALU.mult, ["lnm"], ["lnm2"])
            S.stt("dve", st[:, 34:35], st[:, 9:10], 1.0 / 512, st[:, 33:34], ALU.mult, ALU.subtract, ["st9", "lnm2"], ["lnv"])
            S.ts("dve", st[:, 35:36], st[:, 34:35], LN_EPS, None, ALU.add, None, ["lnv"], ["r2a"])
            rsqrt_chain(st[:, 36:37], st[:, 35:36], st[:, 37:38], "r2")
            S.ts("dve", vg_t[:], vg_t[:], st[:, 32:33], st[:, 36:37], ALU.subtract, ALU.mult, ["vg_t", "lnm", "r2y"], ["vg_t"])
            S.tt("dve", vg_t[:], vg_t[:], lngb[:], ALU.mult, ["vg_t", "lngb"], ["vg_t"])
            S.tt("dve", vn_t[:], vg_t[:], lnbb[:], ALU.add, ["vg_t", "lnbb"], ["vn_t"])
            S.mm([(pD[:, h * 64:(h + 1) * 64], wsT[:, h, :], vn_t[:, h * 64:(h + 1) * 64], True, True) for h in range(8)],
                 ["wsT", "vn_t"], ["pD"])
            S.tt("dve", gm_t[:], pD[:].rearrange("p (h d) -> p h d", h=8), bsb[:].unsqueeze(2).to_broadcast([128, 8, 64]), ALU.add,
                 ["pD", "bsb"], ["gm_t"])
            gmf = gm_t[:].rearrange("p h d -> p (h d)")
            S.tt("dve", gmf, gmf, u_t[:], ALU.mult, ["gm_t", "u_t"], ["gm_t"])
            S.actv(junk[:, 0:512], gmf, AF.Square, ["gm_t"], ["junk", "st11"], accum=st[:, 11:12])
            yield
            S.suffix = "@%d" % par
            S.ts("dve", st[:, 40:42], st[:, 10:12], 1.0 / 512, RS_EPS, ALU.mult, ALU.add, ["st10", "st11"], ["r3a"])
            rsqrt_chain(st[:, 42:44], st[:, 40:42], st[:, 44:46], "r3")
            S.stt("dve", mixed[:, 0:512], a_t[:].rearrange("p h d -> p (h d)"), st[:, 42:43], gmb[:, 0:512], ALU.mult, ALU.mult,
                  ["a_t0", "a_t1", "r3y", "gmb0"], ["mixed0"])
            S.stt("dve", mixed[:, 512:1024], gmf, st[:, 43:44], gmb[:, 512:1024], ALU.mult, ALU.mult,
                  ["gm_t", "r3y", "gmb1"], ["mixed1"])
            S.tr([(pA[:, k, :], mixed[:, k * 128:(k + 1) * 128], identb[:]) for k in range(8)], ["mixed0", "mixed1", "identb"], ["pA"])
            S.copy("act", mixedT[:], pA, ["pA"], ["mixedT"])
            yield
            S.suffix = "@%d" % par
            lst = []
            for n, pp in enumerate((pG, pH)):
                for k in range(8):
                    lst.append((pp[:, :], mixedT[:, k, :], wout[:, k, n * 512:(n + 1) * 512], k == 0, k == 7))
            S.mm(lst, ["mixedT", "wout"], ["pG", "pH"])
            h1s = h1t[gb % 2]
            h1k = f"h1t{gb % 2}"
            S.tt("dve", h1s[:, 0:512], pG[:], xs[:, 0:512], ALU.add, ["pG", xk], [h1k + "a"])
            S.tt("dve", h1s[:, 512:1024], pH[:], xs[:, 512:1024], ALU.add, ["pH", xk], [h1k + "b"])
            S.dma("sp", h1_d[gb * 128:(gb + 1) * 128, :], h1s[:], [h1k + "a", h1k + "b"], [f"h1d_{gb}"], f"h1st{gb % 2}")
            S.actv(junk[:], h1s[:], AF.Square, [h1k + "a", h1k + "b"], ["junk", "st12"], scale=1.0 / 32, accum=st[:, 12:13])
            S.ts("dve", st[:, 48:49], st[:, 12:13], RS_EPS, None, ALU.add, None, ["st12"], ["r4a"])
            rsqrt_chain(st[:, 49:50], st[:, 48:49], st[:, 50:51], "r4")
            S.stt("dve", hn2[:], h1s[:], st[:, 49:50], g2b[:], ALU.mult, ALU.mult, [h1k + "a", h1k + "b", "r4y", "g2b"], ["hn2"])
            S.dma("pool", hn2_d[gb * 128:(gb + 1) * 128, :], hn2[:], ["hn2"], [f"hn2d_{gb}"], f"hn2st{par}")
            yield
            S.suffix = "@%d" % par
            pDE = [pD, pD, pD, pD, pE, pE, pE, pE]
            S.tr([(pDE[k][:, (k % 4) * 128:(k % 4 + 1) * 128], hn2[:, bass.ds(k, 128, step=8)], identf[:]) for k in range(8)],
                 ["hn2", "identf"], ["pD", "pE"])
            S.copy("act", hn2T32[:, 0:4, :], pD[:].rearrange("p (k t) -> p k t", k=4), ["pD"], ["hn2T32a"])
            S.copy("act", hn2T32[:, 4:8, :], pE[:].rearrange("p (k t) -> p k t", k=4), ["pE"], ["hn2T32b"])
            S.mm([(pF[:, 0:36], hn2T32[:, k, :], wr[:, k, :], k == 0, k == 7) for k in range(8)], ["hn2T32a", "hn2T32b"] + WR, ["pF"])
            S.tt("dve", lg[:, b, :], pF[:, 0:36], rbias[:], ALU.add, ["pF", "rb0", "rb1"], [f"lg_{b}"])

        def routing():
            LG = [f"lg_{b}" for b in range(BPP)]
            gl = lg[:, :, 0:4]
            el = lg[:, :, 4:36].rearrange("p b (j e) -> p b j e", j=4)
            gmax = rt[:, :, 0:1]
            ohg = rt[:, :, 1:5]
            gex = rt[:, :, 5:9]
            gsum = rt[:, :, 9:10]
            gw = rt[:, :, 10:11]
            tmp = rt[:, :, 16:48].rearrange("p b (j e) -> p b j e", j=4)
            esel = rt[:, :, 48:56]
            m1 = rt[:, :, 11:12]
            m2 = rt[:, :, 12:13]
            eq1 = rt[:, :, 56:64]
            e2 = rt[:, :, 64:72]
            eq2 = rt[:, :, 72:80]
            dd = rt[:, :, 13:14]
            w1 = rt[:, :, 14:15]
            w2 = rt[:, :, 15:16]
            wn = rt[:, :, 80:88]
            S.red("dve", rt[:, :, 0], gl, ALU.max, LG, ["gmax"])
            S.tt("dve", ohg, gl, gmax.to_broadcast([128, BPP, 4]), ALU.is_equal, LG + ["gmax"], ["ohg"])
            S.tt("dve", gex, gl, gmax.to_broadcast([128, BPP, 4]), ALU.subtract, LG + ["gmax"], ["gex"])
            S.actv(gex, gex, AF.Exp, ["gex"], ["gex"])
            S.red("dve", rt[:, :, 9], gex, ALU.add, ["gex"], ["gsum"])
            S.add("dve", lambda: nc.vector.reciprocal(out=gw, in_=gsum), ["gsum"], ["gw"])
            S.tt("dve", tmp, el, ohg.unsqueeze(3).to_broadcast([128, BPP, 4, 8]), ALU.mult, LG + ["ohg"], ["tmp"])
            S.red("dve", esel, tmp.rearrange("p b j e -> p b e j"), ALU.add, ["tmp"], ["esel"])
            S.red("dve", rt[:, :, 11], esel, ALU.max, ["esel"], ["m1"])
            S.tt("dve", eq1, esel, m1.to_broadcast([128, BPP, 8]), ALU.is_equal, ["esel", "m1"], ["eq1"])
            S.stt("dve", e2, eq1, -1e30, esel, ALU.mult, ALU.add, ["eq1", "esel"], ["e2"])
            S.red("dve", rt[:, :, 12], e2, ALU.max, ["e2"], ["m2"])
            S.tt("dve", eq2, e2, m2.to_broadcast([128, BPP, 8]), ALU.is_equal, ["e2", "m2"], ["eq2"])
            S.tt("dve", dd, m2, m1, ALU.subtract, ["m1", "m2"], ["dd"])
            S.actv(dd, dd, AF.Exp, ["dd"], ["dd"])
            S.ts("dve", w1, dd, 1.0, None, ALU.add, None, ["dd"], ["w1"])
            S.add("dve", lambda: nc.vector.reciprocal(out=w1, in_=w1), ["w1"], ["w1"])
            S.tt("dve", w2, dd, w1, ALU.mult, ["dd", "w1"], ["w2"])
            S.tt("dve", w1, w1, gw, ALU.mult, ["w1", "gw"], ["w1"])
            S.tt("dve", w2, w2, gw, ALU.mult, ["w2", "gw"], ["w2"])
            S.tt("dve", eq1, eq1, w1.to_broadcast([128, BPP, 8]), ALU.mult, ["eq1", "w1"], ["eq1"])
            S.tt("dve", eq2, eq2, w2.to_broadcast([128, BPP, 8]), ALU.mult, ["eq2", "w2"], ["eq2"])
            S.tt("dve", wn, eq1, eq2, ALU.add, ["eq1", "eq2"], ["wn"])
            for j in range(4):
                S.tt("dve", gates[:, :, j * 8:(j + 1) * 8], wn, ohg[:, :, j:j + 1].to_broadcast([128, BPP, 8]), ALU.mult,
                     ["wn", "ohg"], ["gates"])

        def compaction():
            Mf = rt[:, :, 16:48]
            S.add("dve", lambda: nc.vector.tensor_single_scalar(out=Mf, in_=gates[:], scalar=0.0, op=ALU.is_gt), ["gates", "tmp"], ["tmp"])
            S.copy("dve", Mb[:], Mf, ["tmp"], ["Mb"])
            S.add("pool", lambda: nc.gpsimd.memset(Macc[:, 0, :], 0.0), [], ["Macc"])
            for b in range(BPP):
                S.tt("dve", Macc[:, b + 1, :], Macc[:, b, :], Mb[:, b, :], ALU.add, ["Macc", "Mb"], ["Macc"])
            for b in range(BPP):
                bank, bk = (pB, "pB") if (b // 16) % 2 == 0 else (pC, "pC")
                o = bank[:, (b % 16) * 32:(b % 16 + 1) * 32]
                S.mm([(o, utri[:], Mb[:, b, :], True, False), (o, onesb[:], Macc[:, b, :], False, True)], ["utri", "onesb", "Mb", "Macc"], [bk])
                if b % 16 == 15 or b == BPP - 1:
                    b0 = (b // 16) * 16
                    nb = b - b0 + 1
                    S.tt("dve", slf[:, b0:b0 + nb, :], bank[:, 0:nb * 32].rearrange("p (b e) -> p b e", e=32),
                         ecap[:].unsqueeze(1).to_broadcast([128, nb, 32]), ALU.add, [bk, "ecap"], [f"slf{b0}"])
            SLF = [f"slf{b0}" for b0 in range(0, BPP, 16)]
            S.mm([(pD[:, 0:32], onesb[:], Macc[:, BPP, :], True, True)], ["onesb", "Macc"], ["pD"])
            S.copy("dve", cnt_i[:], pD[:, 0:32], ["pD"], ["cnt_i"])
            sel = rt[:, :, 48:80]
            S.stt("dve", sel, slf[:], 1.0, Mf, ALU.add, ALU.mult, SLF + ["tmp"], ["sel"])
            S.ts("dve", sel, sel, -1.0, None, ALU.add, None, ["sel"], ["sel"])
            sA = rt[:, :, 0]
            sB = rt[:, :, 1]
            eqm = rt[:, :, 16:48]
            S.red("dve", sA, sel, ALU.max, ["sel"], ["sA"])
            S.tt("dve", eqm, sel, rt[:, :, 0:1].to_broadcast([128, BPP, 32]), ALU.is_equal, ["sel", "sA", "tmp"], ["tmp"])
            S.tt("dve", slf[:], eqm, gates[:], ALU.mult, ["tmp", "gates"] + SLF, SLF)
            S.red("dve", infoA[:, :, 2], slf[:], ALU.add, SLF, ["infoA1"])
            S.stt("dve", sel, eqm, -1e9, sel, ALU.mult, ALU.add, ["tmp", "sel"], ["sel"])
            S.red("dve", sB, sel, ALU.max, ["sel"], ["sB"])
            S.tt("dve", eqm, sel, rt[:, :, 1:2].to_broadcast([128, BPP, 32]), ALU.is_equal, ["sel", "sB", "tmp"], ["tmp"])
            S.tt("dve", slf[:], eqm, gates[:], ALU.mult, ["tmp", "gates"] + SLF, SLF)
            S.red("dve", infoB[:, :, 2], slf[:], ALU.add, SLF, ["infoB1"])
            S.copy("dve", infoA[:].bitcast(I32)[:, :, 0], tokid[:], ["tokid"], ["infoA0"])
            S.copy("dve", infoB[:].bitcast(I32)[:, :, 0], tokid[:], ["tokid"], ["infoB0"])
            S.copy("dve", infoA[:].bitcast(I32)[:, :, 1], dstt[:, :, 0], ["dstt"], ["infoA0"])
            S.copy("dve", infoB[:].bitcast(I32)[:, :, 1], dstt[:, :, 1], ["dstt"], ["infoB0"])
            S.add("pool", lambda: nc.gpsimd.memset(infoA[:, :, 3], 0.0), [], ["infoA0"])
            S.add("pool", lambda: nc.gpsimd.memset(infoB[:, :, 3], 0.0), [], ["infoB0"])
            S.copy("dve", sAi[:], sA, ["sA"], ["sAi"])
            S.copy("dve", sBi[:], sB, ["sB"], ["sBi"])
            zt = junk[:].bitcast(F32)
            S.add("pool", lambda: nc.gpsimd.memset(zt, 0.0), ["junk@0"], ["junk@0"])
            S.add("pool", lambda: nc.gpsimd.memset(zt.bitcast(I32).rearrange("p (r c) -> p r c", c=4)[:, :, 0], TOK), ["junk@0"], ["junk@0"])
            S.add("pool", lambda: nc.gpsimd.memset(zt.bitcast(I32).rearrange("p (r c) -> p r c", c=4)[:, :, 1], 2 * TOK), ["junk@0"], ["junk@0"])
            iv = info_d.rearrange("(p r) c -> p (r c)", p=128)
            ncol = NE * CAP * 4 // 128
            zw = min(512, ncol)
            for c0 in range(0, ncol, zw):
                S.dma("sp", iv[:, c0:c0 + zw], zt.bitcast(I32)[:, 0:zw], ["junk@0"], ["info"], f"iz{c0 // zw % 8}")
            for b in range(BPP):
                S.add("pool", (lambda b=b: nc.gpsimd.indirect_dma_start(
                    out=info_d[:, :], out_offset=bass.IndirectOffsetOnAxis(ap=sAi[:, b:b + 1], axis=0), in_=infoA[:].bitcast(I32)[:, b, :], in_offset=None)),
                    ["sAi", "infoA0", "infoA1", "info"], [f"infoA_{b}"], dma="scat")
                S.add("pool", (lambda b=b: nc.gpsimd.indirect_dma_start(
                    out=info_d[:, :], out_offset=bass.IndirectOffsetOnAxis(ap=sBi[:, b:b + 1], axis=0), in_=infoB[:].bitcast(I32)[:, b, :], in_offset=None)),
                    ["sBi", "infoB0", "infoB1", "info"], [f"infoB_{b}"], dma="scat")

        H1ALL = [f"h1d_{gb}" for gb in range(NBLK)]
        INFO_ALL = [f"infoA_{b}" for b in range(BPP)] + [f"infoB_{b}" for b in range(BPP)]
        HN2D = [f"hn2d_{gb}" for gb in range(NBLK)]

        def convert_expert(e):
            S.dma("pool", wgb_d[e], wg_d[e], [], [f"wcv_{e}g"], f"wcv{e % 4}")
            S.dma("pool", wub_d[e], wu_d[e], [], [f"wcv_{e}u"], f"wcv{e % 4}")
            S.dma("pool", wdb_d[e], wd_d[e], [], [f"wcv_{e}d"], f"wcv{e % 4}")

        def load_expert(e):
            s = e % 2
            WCV = [f"wcv_{ee}{t}" for ee in range(e % 4, NE, 4) for t in "gud"]
            S.dma("sp", wgt[s][:], wgb_d[e].rearrange("(p k) n -> p k n", k=8), WCV, [f"wgt{s}"], f"wgt{s}")
            S.dma("sp", wup[s][:], wub_d[e].rearrange("(p k) n -> p k n", k=8), WCV, [f"wup{s}"], f"wup{s}")
            S.dma("sp", wdn[s][:], wdb_d[e].rearrange("(m p) n -> p m n", p=128), WCV, [f"wdn{s}"], f"wdn{s}")

        tile_it = [0]

        bc_reg = nc.gpsimd.alloc_register("bc")
        nc.gpsimd.reg_mov(bc_reg, TOK - 1)
        bc2_reg = nc.gpsimd.alloc_register("bc2")
        nc.gpsimd.reg_mov(bc2_reg, 2 * TOK - 1)

        tile_q = {}
        J1_INLINE = min(3, JMAX)

        def tile_A(e, j):
            for _ in tile_A_gen(e, j):
                pass

        def tile_A_gen(e, j):
            s = e % 2
            q = j % 2
            if j < J1_INLINE:
                qx = j
                qb = j if j < 2 else 4
            else:
                qx = q
                qb = 2 + q
            qs = qb if (j >= J1_INLINE or e % 2 == 0) else (5 + j)
            tile_q[(e, j)] = (qb, qs)
            r0 = e * CAP + j * 128
            S.dma("sp", si[qs][:], info_d[r0:r0 + 128, :], INFO_ALL, [f"si{qs}"], f"si{qs}")
            S.add("pool", (lambda qx=qx, qb=qb: nc.gpsimd.indirect_dma_start(
                out=xg[qx], out_offset=None, in_=hn2_d[:, :], in_offset=bass.IndirectOffsetOnAxis(ap=si[qs][:, 0:1], axis=0),
                bounds_check=bc_reg, oob_is_err=False)),
                [f"si{qs}"] + HN2D, [f"xg{qx}"], dma=f"xg{qx}")
            yield
            ptr, ptrk = (pA, "pA") if q == 0 else (pHb, "pH")
            S.tr([(ptr[:, k, :], xg[qx][:, bass.ds(k, 128, step=8)], identb[:]) for k in range(8)], [f"xg{qx}", "identb"], [ptrk])
            yield
            S.copy("act", xgT[qx], ptr, [ptrk], [f"xgT{qx}"])
            yield
            pgu, pguk = (pB, "pB") if q == 0 else (pC, "pC")
            lst = []
            for m in range(2):
                for k in range(8):
                    lst.append((pgu[:, m * 128:(m + 1) * 128], wgt[s][:, k, m * 128:(m + 1) * 128], xgT[qx][:, k, :], k == 0, k == 7))
            for m in range(2):
                for k in range(8):
                    lst.append((pgu[:, 256 + m * 128:256 + (m + 1) * 128], wup[s][:, k, m * 128:(m + 1) * 128], xgT[qx][:, k, :], k == 0, k == 7))
            S.mm(lst, [f"wgt{s}", f"wup{s}", f"xgT{qx}"], [pguk])
            yield
            S.actv(sil[qx][:], pgu[:, 0:256], AF.Silu, [pguk], [f"sil{qx}"])
            S.tt("dve", hid[qx][:].rearrange("p m t -> p (m t)"), pgu[:, 256:512], sil[qx][:], ALU.mult, [pguk, f"sil{qx}"], [f"hid{qx}"])
            yield
            pd = (pD, pE) if q == 0 else (pF, pG)
            pdk = ("pD", "pE") if q == 0 else ("pF", "pG")
            lst = []
            for n in range(2):
                for m in range(2):
                    lst.append((pd[n][:, :], hid[qx][:, m, :], wdn[s][:, m, n * 512:(n + 1) * 512], m == 0, m == 1))
            S.mm(lst, [f"hid{qx}", f"wdn{s}"], list(pdk))
            yield
            S.actv(yt[qb][:, 0:512], pd[0][:], AF.Copy, [pdk[0], f"si{qs}"], [f"yt{qb}a"], scale=si[qs][:, 2:3].bitcast(F32))
            S.ts("dve", yt[qb][:, 512:1024], pd[1][:], si[qs][:, 2:3].bitcast(F32), None, ALU.mult, None, [pdk[1], f"si{qs}"], [f"yt{qb}b"])

        b_cnt = {}

        def tile_B(e, j):
            q, qs = tile_q[(e, j)]
            c = b_cnt.get((e, j), 0)
            b_cnt[(e, j)] = c + 1
            S.add("pool", (lambda q=q, qs=qs: nc.gpsimd.indirect_dma_start(
                out=y2_d[:, :], out_offset=bass.IndirectOffsetOnAxis(ap=si[qs][:, 1:2], axis=0), in_=yt[q], in_offset=None,
                bounds_check=bc2_reg, oob_is_err=False)),
                [f"yt{q}a", f"yt{q}b", f"si{qs}"], [f"y2_{e}_{j}_{c}"], dma=f"yst{q}")
            Y2ALL.append(f"y2_{e}_{j}_{c}")

        Y2ALL = []
        EMAP = {mybir.EngineType.Activation: "act", mybir.EngineType.DVE: "dve", mybir.EngineType.PE: "pe",
                mybir.EngineType.Pool: "pool", mybir.EngineType.SP: "sp"}

        def moe_sparse():
            regs = nc.alloc_registers("n_e")
            RS = [regs, regs]
            NUNC = min(3, JMAX)
            J1 = J1_INLINE

            def cond(e, j, fn):
                if j < NUNC:
                    fn(e, j)
                else:
                    S.begin_if(j * 128, RS[e % 2])
                    fn(e, j)
                    S.end_if()

            def tail(e, j=None):
                if j is None:
                    j = J1
                if j >= JMAX:
                    return
                S.begin_if(j * 128, regs)
                tile_A(e, j)
                if j > J1:
                    tile_B(e, j - 1)
                if j + 1 < JMAX:
                    tail(e, j + 1)
                    S.begin_if((j + 1) * 128, regs, invert=True)
                    tile_B(e, j)
                    S.end_if()
                else:
                    tile_B(e, j)
                S.end_if()

            assert NUNC == J1
            def run_gens(gens):
                hold = [0, 0, 2][:len(gens)]
                while gens:
                    alive = []
                    for gi, g in enumerate(gens):
                        if hold[gi] > 0:
                            hold[gi] -= 1
                            alive.append((g, hold[gi]))
                            continue
                        try:
                            next(g)
                            alive.append((g, 0))
                        except StopIteration:
                            pass
                    gens = [g for g, _ in alive]
                    hold = [h for _, h in alive]

            def front(e):
                gens = [tile_A_gen(e, jj) for jj in range(NUNC)]
                for g in gens:
                    next(g)
                return gens

            gens = front(0)
            for e in range(NE_RUN):
                for r in regs:
                    S.add(EMAP[r.engine], (lambda r=r, e=e: nc.reg_load(r, cnt_i[0:1, e:e + 1])), ["cnt_i"], [])
                if e + 1 < NE_RUN:
                    load_expert(e + 1)
                run_gens(gens)
                tail(e)
                if e + 1 < NE_RUN:
                    gens = front(e + 1)
                for jj in range(J1):
                    tile_B(e, jj)

        NCL = 3

        def combine_block(b):
            q = b % NCL
            S.dma("sp", hc[q], h1_d[b * 128:(b + 1) * 128, :], H1ALL + Y2ALL, [f"hc{q}"], f"hc{q}")
            S.dma("sp", yc[q], y2_d[2 * b * 128:2 * (b + 1) * 128, :].rearrange("(p k) d -> p k d", k=2), Y2ALL, [f"yc{q}"], f"yc{q}")
            yield
            S.tt("pool", yc[q][:, 0, :], yc[q][:, 0, :], yc[q][:, 1, :], ALU.add, [f"yc{q}"], [f"yc{q}"])
            yield
            S.tt("dve", hc[q], hc[q], yc[q][:, 0, :], ALU.add, [f"hc{q}", f"yc{q}"], [f"hc{q}"])
            yield
            k0 = 40 + 4 * q
            S.actv(junk[:], hc[q], AF.Square, [f"hc{q}"], ["junk@0", f"f{q}s"], scale=1.0 / 32, accum=st[:, k0:k0 + 1])
            yield
            S.ts("dve", st[:, k0 + 1:k0 + 2], st[:, k0:k0 + 1], RS_EPS, None, ALU.add, None, [f"f{q}s"], [f"f{q}a"])
            rsqrt_chain(st[:, k0 + 2:k0 + 3], st[:, k0 + 1:k0 + 2], st[:, k0 + 3:k0 + 4], f"f{q}")
            S.stt("dve", hc[q], hc[q], st[:, k0 + 2:k0 + 3], gfb[:], ALU.mult, ALU.mult, [f"hc{q}", f"f{q}y", "gfb"], [f"hc{q}"])
            yield
            S.dma("sp", out_d[b * 128:(b + 1) * 128, :], hc[q], [f"hc{q}"], [], f"ost{q}")

        def combine():
            def clane(start):
                for b in range(start, NBLK, NCL):
                    yield from combine_block(b)
            lanes = [clane(i) for i in range(NCL)]
            for i in range(NCL):
                for _ in range(2 * (NCL - 1 - i)):
                    next(lanes[i])
            while lanes:
                for ln in list(lanes):
                    try:
                        next(ln)
                    except StopIteration:
                        lanes.remove(ln)

        for _ in block(-1, halo=True):
            pass

        def lane(start):
            for gb in range(start, NBLK, 2):
                if gb < NE:
                    sv = S.suffix
                    S.suffix = ""
                    convert_expert(gb)
                    S.suffix = sv
                yield from block(gb)

        lanes = [lane(0), lane(1)]
        for _ in range(3):
            next(lanes[0])
        while lanes:
            for ln in list(lanes):
                try:
                    next(ln)
                except StopIteration:
                    lanes.remove(ln)
        S.suffix = ""
        for e in range(NBLK, NE):
            convert_expert(e)
        routing()
        compaction()
        S.add("pool", lambda: nc.gpsimd.memset(st[:, 60:61], 0.0), ["win", "wout"],
              ["win", "wout", "xg0", "xg1", "xgT0", "xgT1", "yt0a", "yt0b", "yt1a", "yt1b", "yt2a", "yt2b", "yt3a", "yt3b", "yt4a", "yt4b", "xg2", "xgT2", "yc0", "yc1", "yc2", "hc0", "hc1", "hc2"])
        S.add("pool", lambda: nc.gpsimd.memset(woutf[:, 0:2048], 0.0), ["xg0", "xg1"], ["xg0", "xg1"])
        S.add("pool", lambda: nc.gpsimd.memset(xg[2], 0.0), ["xg2"], ["xg2"])
        load_expert(0)
        moe_sparse()
        combine()
        S.emit()
        globals()['_LAST_NDUMMY'] = S.ndummy
    return nc


_NC_CACHE = {}


def _prep_inputs(inputs):
    f = lambda a: np.ascontiguousarray(np.asarray(a, dtype=np.float32))
    x = f(inputs["x"])
    B, Sq, _ = x.shape
    w_in = f(inputs["w_in"])[0]
    perm = []
    for c in range(4):
        perm += list(range(c * 64, c * 64 + 64)) + list(range((4 + c) * 64, (4 + c) * 64 + 64))
    perm += list(range(512, INP))
    w_in_p = np.ascontiguousarray(w_in[:, perm])
    s_idx = np.arange(128)[:, None]
    q_idx = np.arange(128)[None, :]
    mcur = (s_idx <= q_idx).astype(np.float32)
    mprev = (s_idx > q_idx).astype(np.float32)
    tril = (q_idx <= s_idx).astype(np.float32)
    common = {
        "mix_norm_g": f(inputs["mix_norm_g"]).reshape(1, D),
        "w_in": w_in_p,
        "attn_sinks": f(inputs["attn_sinks"]).reshape(1, 8),
        "w_spatial": f(inputs["w_spatial"])[0],
        "b_spatial": f(inputs["b_spatial"])[0],
        "gmlp_ln_g": f(inputs["gmlp_ln_g"]).reshape(1, 512),
        "gmlp_ln_b": f(inputs["gmlp_ln_b"]).reshape(1, 512),
        "attn_out_g": f(inputs["attn_out_g"]).reshape(1, 512),
        "gmlp_out_g": f(inputs["gmlp_out_g"]).reshape(1, 512),
        "w_out": f(inputs["w_out"])[0],
        "ffn_norm_g": f(inputs["ffn_norm_g"]).reshape(1, D),
        "w_group_router": f(inputs["w_group_router"])[0],
        "b_group_router": f(inputs["b_group_router"]).reshape(1, 4),
        "w_expert_router": f(inputs["w_expert_router"])[0],
        "b_expert_router": f(inputs["b_expert_router"]).reshape(1, 32),
        "w_gate": f(inputs["w_gate"])[0],
        "w_up": f(inputs["w_up"])[0],
        "w_down": f(inputs["w_down"])[0],
        "final_norm_g": f(inputs["final_norm_g"]).reshape(1, D),
    }
    xf = x.reshape(B * Sq, D)
    NBLK_H = TOK // 128
    in_maps = []
    for c in range(NCORES):
        t0 = c * TOK
        first = (t0 % Sq) == 0
        masks = np.zeros((128, 4, 512), np.float32)
        masks[:, 0, :] = np.tile(mcur, (1, 4))
        masks[:, 1, :] = np.tile(mprev, (1, 4))
        masks[:, 2, :] = 0.0 if first else np.tile(mprev, (1, 4))
        masks[:, 3, 0:128] = tril
        xh = np.zeros((128, D), np.float32) if first else xf[t0 - 128:t0]
        m = dict(common)
        m["x"] = np.ascontiguousarray(xf[t0:t0 + TOK])
        m["xh"] = np.ascontiguousarray(xh)
        m["masks"] = masks
        m["tokid"] = (np.arange(NBLK_H)[None, :] * 128 + np.arange(128)[:, None]).astype(np.int32)
        m["dst"] = np.stack([2 * m["tokid"], 2 * m["tokid"] + 1], axis=-1).astype(np.int32)
        m["ecap"] = (np.arange(32, dtype=np.float32) * TOK).reshape(1, 32)
        in_maps.append(m)
    return in_maps, (B, Sq)


def kernel(**inputs):
    in_maps, (B, Sq) = _prep_inputs(inputs)
    if "nc" not in _NC_CACHE:
        _NC_CACHE["nc"] = build_nc()
    nc = _NC_CACHE["nc"]
    res = run_bass_kernel_spmd(nc, in_maps, core_ids=list(range(NCORES)))
    out = np.concatenate([np.asarray(r["out"]) for r in res.results], axis=0)
    return out.reshape(B, Sq, D).astype(np.float32)
```
